# Optimizing a Trainium2 kernel written in Bass

```python
import numpy as np
import jax
import jax.numpy as jnp
from jax import lax

D_MODEL = 1024
BATCH = 16
SEQ = 2048
DEPTH = 1

GRID_W = 64
CTX_LEN = 256
N_MOD = 6
EPS = 1e-6

RW_HEAD_DIM = 64
RW_HEADS = (D_MODEL // 2) // RW_HEAD_DIM
RW_WIDTH = RW_HEADS * RW_HEAD_DIM
RW_DECAY_LORA = 64
RW_AAA_LORA = 64
RW_GATE_LORA = 128
RW_GN_EPS = 1e-5 * RW_HEAD_DIM
CONV_K = 3

HG_KEY_DIM = 64
HG_VAL_DIM = 64
HG_HEADS = (D_MODEL // 2) // HG_VAL_DIM
HG_KWIDTH = HG_HEADS * HG_KEY_DIM
HG_VWIDTH = HG_HEADS * HG_VAL_DIM
HG_CHUNK = 32

D_MIX = RW_WIDTH + HG_VWIDTH
IN_SPLITS = (RW_WIDTH, RW_WIDTH, RW_WIDTH, 2 * RW_DECAY_LORA, 2 * RW_AAA_LORA, RW_GATE_LORA,
             HG_KWIDTH, HG_KWIDTH, HG_KWIDTH, HG_VWIDTH, HG_VWIDTH)
P_IN = sum(IN_SPLITS)

N_EXPERTS = 64
N_GROUPS = 8
TOPK_GROUPS = 4
TOP_K = 8
D_EXPERT = D_MODEL // 4
D_SHARED = D_MODEL // 4
ROUTE_SCALE = 2.5
MOE_BLOCK = 256

kernel_name = 'hybrid_rwkv7_hgrn2_moe_dit_layer'


def _rmsnorm(x, g):
    xf = x.astype(jnp.float32)
    y = xf * lax.rsqrt(jnp.mean(xf * xf, axis=-1, keepdims=True) + EPS)
    return (y * g.astype(jnp.float32)).astype(x.dtype)


def _split_cols(u):
    idx = [int(i) for i in np.cumsum(IN_SPLITS)[:-1]]
    return jnp.split(u, idx, axis=-1)


def _heads(t, h, d):
    return t.reshape(t.shape[0], t.shape[1], h, d).astype(jnp.float32)


def _conv_latent(u, kern, rows):
    b, l, ch = u.shape
    u2 = u.reshape(b, rows, GRID_W, ch)
    out = lax.conv_general_dilated(u2, kern[:, :, None, :].astype(u.dtype), window_strides=(1, 1),
                                   padding='SAME', dimension_numbers=('NHWC', 'HWIO', 'NHWC'),
                                   feature_group_count=ch)
    return out.reshape(b, l, ch)


def _conv_context(u, kern):
    ch = u.shape[-1]
    return lax.conv_general_dilated(u, kern[CONV_K // 2][:, None, :].astype(u.dtype), window_strides=(1,),
                                    padding='SAME', dimension_numbers=('NWC', 'WIO', 'NWC'),
                                    feature_group_count=ch)


def _rwkv7_scan(r, w, k, v, a, b, s0, reverse, emit):
    def step(s, inp):
        r_t, w_t, k_t, v_t, a_t, b_t = inp
        sa = jnp.einsum('bhvk,bhk->bhv', s, a_t)
        s = s * w_t[:, :, None, :] + sa[..., None] * b_t[:, :, None, :] + v_t[..., None] * k_t[:, :, None, :]
        y = jnp.einsum('bhvk,bhk->bhv', s, r_t) if emit else None
        return s, y
    xs = tuple(jnp.moveaxis(t, 1, 0) for t in (r, w, k, v, a, b))
    s_fin, ys = lax.scan(step, s0, xs, reverse=reverse)
    return (jnp.moveaxis(ys, 0, 1) if emit else None), s_fin


def _rwkv7_mix(cols, conv_fn, s0, w0, w2, a0, a2, g2, k_k, k_a, r_k, lnx_g, lnx_b, emit):
    r, k, v, wd, ad, gd = cols
    r, k, v = jnp.split(conv_fn(jnp.concatenate([r, k, v], axis=-1)), 3, axis=-1)
    bsz, l, _ = r.shape
    rh = _heads(r, RW_HEADS, RW_HEAD_DIM)
    vh = _heads(v, RW_HEADS, RW_HEAD_DIM)
    kk = _heads(k * k_k, RW_HEADS, RW_HEAD_DIM)
    kk = kk / jnp.maximum(jnp.sqrt(jnp.sum(kk * kk, axis=-1, keepdims=True)), 1e-12)
    ys, bonus, states = [], [], []
    for d in range(2):
        wl = wd[..., d * RW_DECAY_LORA:(d + 1) * RW_DECAY_LORA]
        al = ad[..., d * RW_AAA_LORA:(d + 1) * RW_AAA_LORA]
        w = -jax.nn.softplus(-(w0[d] + jnp.tanh(wl) @ w2[d])) - 0.5
        decay = jnp.exp(-jnp.exp(_heads(w, RW_HEADS, RW_HEAD_DIM)))
        a_flat = jax.nn.sigmoid(a0[d] + al @ a2[d])
        a = _heads(a_flat, RW_HEADS, RW_HEAD_DIM)
        kd = _heads(k * (1.0 + (a_flat - 1.0) * k_a), RW_HEADS, RW_HEAD_DIM)
        y, s = _rwkv7_scan(rh, decay, kd, vh, -kk, kk * a, s0[d], d == 1, emit)
        states.append(s)
        if emit:
            ys.append(y)
            bonus.append(jnp.sum(rh * kd * r_k.astype(jnp.float32), axis=-1, keepdims=True) * vh)
    if not emit:
        return None, (states[0], states[1])
    y = ys[0] + ys[1]
    mu = jnp.mean(y, axis=-1, keepdims=True)
    var = jnp.mean(jnp.square(y - mu), axis=-1, keepdims=True)
    yn = ((y - mu) * lax.rsqrt(var + RW_GN_EPS)).reshape(bsz, l, RW_WIDTH) * lnx_g + lnx_b
    yn = yn + (bonus[0] + bonus[1]).reshape(bsz, l, RW_WIDTH)
    g = jax.nn.sigmoid(gd) @ g2
    return (yn * g).astype(r.dtype), (states[0], states[1])


def _gla_chunked(q, k, v, logf, s0, emit):
    bsz, l, h, _ = q.shape
    dv = v.shape[-1]
    n = l // HG_CHUNK
    blk = lambda t: jnp.moveaxis(t.reshape(bsz, n, HG_CHUNK, h, t.shape[-1]), 3, 1)
    q, k, v, logf = blk(q), blk(k), blk(v), blk(logf)
    cum = jnp.cumsum(logf, axis=3)
    last = cum[:, :, :, -1:, :]
    kv = jnp.einsum('bhnjk,bhnjv->bhnkv', k * jnp.exp(last - cum), v)
    chunk_decay = jnp.exp(last[:, :, :, 0, :])
    def step(s, inp):
        dc, kvc = inp
        return dc[..., None] * s + kvc, s
    s_fin, s_prev = lax.scan(step, s0, (jnp.moveaxis(chunk_decay, 2, 0), jnp.moveaxis(kv, 2, 0)))
    if not emit:
        return None, s_fin
    s_prev = jnp.moveaxis(s_prev, 0, 2)
    ref = cum[:, :, :, HG_CHUNK // 2:HG_CHUNK // 2 + 1, :]
    scores = jnp.einsum('bhnik,bhnjk->bhnij', q * jnp.exp(cum - ref), k * jnp.exp(ref - cum))
    causal = jnp.tril(jnp.ones((HG_CHUNK, HG_CHUNK), dtype=bool))
    scores = jnp.where(causal, scores, 0.0)
    o = (jnp.einsum('bhnij,bhnjv->bhniv', scores, v)
         + jnp.einsum('bhnik,bhnkv->bhniv', q * jnp.exp(cum), s_prev))
    return jnp.moveaxis(o, 1, 3).reshape(bsz, l, h, dv), s_fin


def _hgrn2_mix(cols, s0, lb, norm_g, emit):
    q, f_fw, f_bw, i, g = cols
    bsz, l, _ = q.shape
    qh = _heads(jax.nn.silu(q), HG_HEADS, HG_KEY_DIM) * (HG_KEY_DIM ** -0.5)
    ih = _heads(i, HG_HEADS, HG_VAL_DIM)
    outs, states = [], []
    for d, f_raw in enumerate((f_fw, f_bw)):
        fr = _heads(f_raw, HG_HEADS, HG_KEY_DIM)
        lbd = lb[d].reshape(HG_HEADS, HG_KEY_DIM)
        logf = jnp.log(lbd + (1.0 - lbd) * jax.nn.sigmoid(fr))
        kf = (1.0 - lbd) * jax.nn.sigmoid(-fr)
        args = (qh, kf, ih, logf)
        if d == 1:
            args = tuple(jnp.flip(t, axis=1) for t in args)
        o, s = _gla_chunked(args[0], args[1], args[2], args[3], s0[d], emit)
        states.append(s)
        if emit:
            outs.append(jnp.flip(o, axis=1) if d == 1 else o)
    if not emit:
        return None, (states[0], states[1])
    o = outs[0] + outs[1]
    o = o * lax.rsqrt(jnp.mean(o * o, axis=-1, keepdims=True) + EPS) * norm_g.astype(jnp.float32)
    o = o.reshape(bsz, l, HG_VWIDTH) * jax.nn.silu(g)
    return o.astype(g.dtype), (states[0], states[1])


def _swiglu(x, wg, wu, wd):
    return (jax.nn.silu(x @ wg) * (x @ wu)) @ wd


def _moe(h, router_w, router_b, wg, wu, wd, sg, su, sd):
    t = h.shape[0]
    scores = jax.nn.sigmoid(h.astype(jnp.float32) @ router_w.astype(jnp.float32))
    sel = scores + router_b.astype(jnp.float32)
    grp = sel.reshape(t, N_GROUPS, N_EXPERTS // N_GROUPS)
    grp_score = jnp.sum(lax.top_k(grp, 2)[0], axis=-1)
    _, top_g = lax.top_k(grp_score, TOPK_GROUPS)
    gmask = jnp.any(top_g[:, :, None] == jnp.arange(N_GROUPS)[None, None, :], axis=1)
    sel = jnp.where(jnp.repeat(gmask, N_EXPERTS // N_GROUPS, axis=1), sel, -jnp.inf)
    _, top_e = lax.top_k(sel, TOP_K)
    gate = jnp.take_along_axis(scores, top_e, axis=1)
    gate = gate / jnp.sum(gate, axis=-1, keepdims=True) * ROUTE_SCALE
    n_assign = t * TOP_K
    flat_e = top_e.reshape(-1)
    flat_tok = jnp.arange(n_assign, dtype=jnp.int32) // TOP_K
    flat_gate = gate.reshape(-1)
    order = jnp.argsort(flat_e)
    e_sorted, tok_sorted, gate_sorted = flat_e[order], flat_tok[order], flat_gate[order]
    counts = jnp.bincount(flat_e, length=N_EXPERTS)
    starts = jnp.cumsum(counts) - counts
    padded = (counts + MOE_BLOCK - 1) // MOE_BLOCK * MOE_BLOCK
    pad_end = jnp.cumsum(padded)
    pad_start = pad_end - padded
    dest = pad_start[e_sorted] + (jnp.arange(n_assign, dtype=jnp.int32) - starts[e_sorted])
    n_blocks = -(-n_assign // MOE_BLOCK) + N_EXPERTS
    tok_buf = jnp.zeros((n_blocks * MOE_BLOCK,), jnp.int32).at[dest].set(tok_sorted).reshape(n_blocks, MOE_BLOCK)
    gate_buf = jnp.zeros((n_blocks * MOE_BLOCK,), jnp.float32).at[dest].set(gate_sorted).reshape(n_blocks, MOE_BLOCK)
    blk_e = jnp.minimum(jnp.searchsorted(pad_end, jnp.arange(n_blocks, dtype=jnp.int32) * MOE_BLOCK, side='right'),
                        N_EXPERTS - 1)
    def step(out, inp):
        tok, gw, e = inp
        y = _swiglu(h[tok], wg[e], wu[e], wd[e]) * gw[:, None].astype(h.dtype)
        return out.at[tok].add(y), None
    routed, _ = lax.scan(step, jnp.zeros_like(h), (tok_buf, gate_buf, blk_e))
    return _swiglu(h, sg, su, sd) + routed


def setup_inputs(seed: int = 0) -> dict:
    key = jax.random.key(seed)
    ks = jax.random.split(key, 32)
    nrm = lambda k, shape, scale: jax.random.normal(k, shape, jnp.float32) * scale
    d = D_MODEL
    return {
        'x': nrm(ks[0], (BATCH, SEQ, d), 1.0),
        'c': nrm(ks[1], (BATCH, d), 1.0),
        'ctx': nrm(ks[2], (BATCH, CTX_LEN, d), 1.0),
        'c_ctx': nrm(ks[3], (d,), 1.0),
        'w_mod': nrm(ks[4], (DEPTH, d, N_MOD * d), 0.5 * d ** -0.5),
        'b_mod': nrm(ks[5], (DEPTH, N_MOD * d), 0.02),
        'norm1_g': 1.0 + nrm(ks[6], (DEPTH, d), 0.05),
        'norm2_g': 1.0 + nrm(ks[7], (DEPTH, d), 0.05),
        'w_in': nrm(ks[8], (DEPTH, d, P_IN), d ** -0.5),
        'rw_conv': jnp.zeros((DEPTH, CONV_K, CONV_K, 3 * RW_WIDTH), jnp.float32).at[:, CONV_K // 2, CONV_K // 2, :].set(1.0)
                   + nrm(ks[9], (DEPTH, CONV_K, CONV_K, 3 * RW_WIDTH), 0.15),
        'rw_w0': jax.random.uniform(ks[10], (DEPTH, 2, RW_WIDTH), jnp.float32, -6.0, -1.0),
        'rw_w2': nrm(ks[11], (DEPTH, 2, RW_DECAY_LORA, RW_WIDTH), 0.1),
        'rw_a0': nrm(ks[12], (DEPTH, 2, RW_WIDTH), 0.3),
        'rw_a2': nrm(ks[13], (DEPTH, 2, RW_AAA_LORA, RW_WIDTH), 0.1),
        'rw_g2': nrm(ks[14], (DEPTH, RW_GATE_LORA, RW_WIDTH), RW_GATE_LORA ** -0.5),
        'rw_k_k': 0.85 + nrm(ks[15], (DEPTH, RW_WIDTH), 0.05),
        'rw_k_a': 1.0 + nrm(ks[16], (DEPTH, RW_WIDTH), 0.05),
        'rw_r_k': nrm(ks[17], (DEPTH, RW_HEADS, RW_HEAD_DIM), 0.1),
        'rw_lnx_g': 1.0 + nrm(ks[18], (DEPTH, RW_WIDTH), 0.05),
        'rw_lnx_b': nrm(ks[19], (DEPTH, RW_WIDTH), 0.02),
        'hg_lb_logits': nrm(ks[20], (2, DEPTH + 1, HG_KWIDTH), 0.5),
        'hg_norm_g': 1.0 + nrm(ks[21], (DEPTH, HG_VAL_DIM), 0.05),
        'w_out': nrm(ks[22], (DEPTH, D_MIX, d), D_MIX ** -0.5),
        'router_w': nrm(ks[23], (DEPTH, d, N_EXPERTS), d ** -0.5),
        'router_b': nrm(ks[24], (DEPTH, N_EXPERTS), 0.01),
        'exp_w_gate': nrm(ks[25], (DEPTH, N_EXPERTS, d, D_EXPERT), d ** -0.5),
        'exp_w_up': nrm(ks[26], (DEPTH, N_EXPERTS, d, D_EXPERT), d ** -0.5),
        'exp_w_down': nrm(ks[27], (DEPTH, N_EXPERTS, D_EXPERT, d), D_EXPERT ** -0.5),
        'sh_w_gate': nrm(ks[28], (DEPTH, d, D_SHARED), d ** -0.5),
        'sh_w_up': nrm(ks[29], (DEPTH, d, D_SHARED), d ** -0.5),
        'sh_w_down': nrm(ks[30], (DEPTH, D_SHARED, d), D_SHARED ** -0.5),
        'final_norm_g': 1.0 + nrm(ks[31], (d,), 0.05),
    }


def reference(x, c, ctx, c_ctx, w_mod, b_mod, norm1_g, norm2_g, w_in, rw_conv, rw_w0, rw_w2, rw_a0, rw_a2,
              rw_g2, rw_k_k, rw_k_a, rw_r_k, rw_lnx_g, rw_lnx_b, hg_lb_logits, hg_norm_g, w_out, router_w,
              router_b, exp_w_gate, exp_w_up, exp_w_down, sh_w_gate, sh_w_up, sh_w_down, final_norm_g):
    bsz, seq, d = x.shape
    rows = seq // GRID_W
    ctx_len = ctx.shape[1]
    lb_all = jnp.cumsum(jax.nn.softmax(hg_lb_logits.astype(jnp.float32), axis=1), axis=1)
    zero_rw = jnp.zeros((bsz, RW_HEADS, RW_HEAD_DIM, RW_HEAD_DIM), jnp.float32)
    zero_hg = jnp.zeros((bsz, HG_HEADS, HG_KEY_DIM, HG_VAL_DIM), jnp.float32)
    for l in range(DEPTH):
        last = l == DEPTH - 1
        mod_lat = (jax.nn.silu(c) @ w_mod[l] + b_mod[l])[:, None, :]
        mod_ctx = (jax.nn.silu(c_ctx) @ w_mod[l] + b_mod[l])[None, None, :]
        sh1, sc1, g1, sh2, sc2, g2 = jnp.split(mod_lat, N_MOD, axis=-1)
        csh1, csc1, cg1, csh2, csc2, cg2 = jnp.split(mod_ctx, N_MOD, axis=-1)
        h_lat = _rmsnorm(x, norm1_g[l]) * (1.0 + sc1) + sh1
        h_ctx = _rmsnorm(ctx, norm1_g[l]) * (1.0 + csc1) + csh1
        cols_lat = _split_cols(h_lat @ w_in[l])
        cols_ctx = _split_cols(h_ctx @ w_in[l])
        kern = rw_conv[l]
        rw_p = (rw_w0[l], rw_w2[l], rw_a0[l], rw_a2[l], rw_g2[l], rw_k_k[l], rw_k_a[l], rw_r_k[l],
                rw_lnx_g[l], rw_lnx_b[l])
        lb_l = lb_all[:, l]
        y_rw_ctx, s_rw = _rwkv7_mix(cols_ctx[:6], lambda u: _conv_context(u, kern), (zero_rw, zero_rw), *rw_p,
                                    emit=not last)
        y_rw_lat, _ = _rwkv7_mix(cols_lat[:6], lambda u: _conv_latent(u, kern, rows), s_rw, *rw_p, emit=True)
        y_hg_ctx, s_hg = _hgrn2_mix(cols_ctx[6:], (zero_hg, zero_hg), lb_l, hg_norm_g[l], emit=not last)
        y_hg_lat, _ = _hgrn2_mix(cols_lat[6:], s_hg, lb_l, hg_norm_g[l], emit=True)
        x = x + g1 * (jnp.concatenate([y_rw_lat, y_hg_lat], axis=-1) @ w_out[l])
        moe_p = (router_w[l], router_b[l], exp_w_gate[l], exp_w_up[l], exp_w_down[l],
                 sh_w_gate[l], sh_w_up[l], sh_w_down[l])
        h2_lat = _rmsnorm(x, norm2_g[l]) * (1.0 + sc2) + sh2
        if last:
            x = x + g2 * _moe(h2_lat.reshape(-1, d), *moe_p).reshape(bsz, seq, d)
        else:
            ctx = ctx + cg1 * (jnp.concatenate([y_rw_ctx, y_hg_ctx], axis=-1) @ w_out[l])
            h2_ctx = _rmsnorm(ctx, norm2_g[l]) * (1.0 + csc2) + csh2
            tokens = jnp.concatenate([h2_ctx, h2_lat], axis=1).reshape(-1, d)
            f = _moe(tokens, *moe_p).reshape(bsz, ctx_len + seq, d)
            ctx = ctx + cg2 * f[:, :ctx_len]
            x = x + g2 * f[:, ctx_len:]
    return _rmsnorm(x, final_norm_g)
```

```python
import contextlib
import numpy as np
import concourse.bass as bass
import concourse.mybir as mybir
from concourse.bass_utils import run_bass_kernel_spmd

F32 = mybir.dt.float32
BF16 = mybir.dt.bfloat16
AF = mybir.ActivationFunctionType
ALU = mybir.AluOpType
AX = mybir.AxisListType

NB = 2
SEQ = 2048
CTX = 256
TT = SEQ + CTX
D = 1024
LAM = float(np.exp(-0.5))
DEBUG = {}


class _Stop(Exception):
    pass


class _Rec:
    def __init__(self):
        self.call = None

    def __getattr__(self, name):
        def m(*a, **k):
            self.call = (name, a, k)
            return self
        return m


class Prog:
    ENGS = ("tensor", "vector", "scalar", "gpsimd", "sync")
    NDMA = 40

    def __init__(self, nc):
        self.nc = nc
        self.items = {e: [] for e in self.ENGS}
        self.count = {e: 0 for e in self.ENGS}
        self.seen = {e: {} for e in self.ENGS}
        self.state = {}
        self.names = {}
        self.dma_rr = {"sync": 0, "gpsimd": 0}
        self.dma_rng = {"sync": (0, 28), "gpsimd": (28, 40)}
        self.dma_cnt = [0] * self.NDMA
        self.out_events = []

    def _keys(self, k):
        if not isinstance(k, tuple):
            k = (k, None)
        name, sub = k
        subs = self.names.setdefault(name, set())
        if sub is None:
            return [(name, s) for s in subs | {None}]
        subs.add(sub)
        return [(name, sub), (name, None)]

    def _deps(self, reads, writes):
        ev = []
        for k in reads:
            for kk in self._keys(k):
                st = self.state.get(kk)
                if st and st[0] is not None:
                    ev.append(st[0])
        for k in writes:
            for kk in self._keys(k):
                st = self.state.get(kk)
                if st:
                    if st[0] is not None:
                        ev.append(st[0])
                    ev.extend(st[1])
        return ev

    def _commit(self, reads, writes, event):
        for k in reads:
            kk = k if isinstance(k, tuple) else (k, None)
            self._keys(k)
            st = self.state.setdefault(kk, [None, []])
            st[1].append(event)
            if len(st[1]) > 64:
                best = {}
                for (sk, v) in st[1]:
                    if best.get(sk, 0) < v:
                        best[sk] = v
                st[1] = list(best.items())
        for k in writes:
            kk = k if isinstance(k, tuple) else (k, None)
            if kk[1] is None:
                for s in self._keys(k):
                    self.state[s] = [event, []]
            else:
                self._keys(k)
                self.state[kk] = [event, []]

    def _waits(self, eng, events):
        need = {}
        for (sk, val) in events:
            if sk == ("e", "tensor") and eng == "tensor":
                continue
            if self.seen[eng].get(sk, 0) >= val:
                continue
            need[sk] = max(need.get(sk, 0), val)
        for sk, val in need.items():
            self.seen[eng][sk] = val
        return list(need.items())

    def _lim(self):
        import os
        self.nemit = getattr(self, "nemit", 0) + 1
        lim = int(os.environ.get("PLIMIT", "0"))
        if lim and self.nemit > lim:
            self.disabled = True

    _BANK = ("pT", "bA", "bB", "bC", "pA", "pm", "pR", "pG", "pU")

    def _norm(self, ks):
        return [k[0] if isinstance(k, tuple) and k[0][:2] in self._BANK else k for k in ks]

    def op(self, eng, fn, reads=(), writes=()):
        reads, writes = self._norm(reads), self._norm(writes)
        self._lim()
        if getattr(self, "disabled", False):
            return None
        ev = self._deps(reads, writes)
        waits = self._waits(eng, ev)
        self.count[eng] += 1
        event = (("e", eng), self.count[eng])
        rec = _Rec()
        fn(rec)
        self.items[eng].append((waits, rec.call, ("e", eng), 1))
        self._commit(reads, writes, event)
        return event

    def dma(self, fn, reads=(), writes=(), eng="sync", is_output=False):
        self._lim()
        if getattr(self, "disabled", False):
            return None
        ev = self._deps(reads, writes)
        lo, hi = self.dma_rng[eng]
        i = lo + self.dma_rr[eng]
        self.dma_rr[eng] = (self.dma_rr[eng] + 1) % (hi - lo)
        sk = ("d", i)
        if self.dma_cnt[i] > 0:
            ev.append((sk, self.dma_cnt[i]))
        waits = self._waits(eng, ev)
        self.dma_cnt[i] += 16
        event = (sk, self.dma_cnt[i])
        rec = _Rec()
        fn(rec)
        self.items[eng].append((waits, rec.call, sk, 16))
        self._commit(reads, writes, event)
        if is_output:
            self.out_events.append(event)
        return event

    def barrier(self):
        if getattr(self, "disabled", False):
            return
        evs = [(("e", e), self.count[e]) for e in self.ENGS if self.count[e] > 0]
        evs += [(("d", i), self.dma_cnt[i]) for i in range(self.NDMA) if self.dma_cnt[i] > 0]
        for e in self.ENGS:
            w = self._waits(e, evs)
            if w:
                self.items[e].append((w, None, None, 0))
        self.state = {}
        self.names = {}

    def finish(self):
        evs = list(self.out_events)
        evs += [(("d", i), self.dma_cnt[i]) for i in range(self.NDMA) if self.dma_cnt[i] > 0]
        evs += [(("e", e), self.count[e]) for e in self.ENGS if self.count[e] > 0]
        w = self._waits("sync", evs)
        if w:
            self.items["sync"].append((w, None, None, 0))

    def replay(self, stack):
        nc = self.nc
        sems = {}
        for e in self.ENGS:
            sems[("e", e)] = stack.enter_context(nc.semaphore("pe_" + e))
        for i in range(self.NDMA):
            sems[("d", i)] = stack.enter_context(nc.semaphore("pd_%d" % i))
        block = stack.enter_context(nc.Block())
        items = self.items

        def run(engname):
            def body(eng):
                for (waits, fn, sk, inc) in items[engname]:
                    for (wk, val) in waits:
                        eng.wait_ge(sems[wk], val)
                    if fn is not None:
                        ins = getattr(eng, fn[0])(*fn[1], **fn[2])
                        ins.then_inc(sems[sk], inc)
            return body

        block.tensor(run("tensor"))
        block.vector(run("vector"))
        block.scalar(run("scalar"))
        block.gpsimd(run("gpsimd"))
        block.sync(run("sync"))


PV = {}
_o = 0
for _n, _w in (("w0", 8), ("a0", 8), ("k_k", 4), ("k_a", 4), ("omka", 4), ("r_k", 4), ("lnx_g", 4), ("lnx_b", 4),
               ("lbl", 16), ("hgn", 1), ("n1g", 8), ("n2g", 8)):
    PV[_n] = _o
    _o += _w
NPV = _o


def build_program(stop_after=None):
    nc = bass.Bass("TRN2", target_bir_lowering=False)
    dt_in = lambda name, shape: nc.dram_tensor(name, shape, F32, kind="ExternalInput").ap()
    x_in = dt_in("x_in", [NB, SEQ, D])
    ctx_in = dt_in("ctx_in", [NB, CTX, D])
    cvec_in = dt_in("cvec", [128, 8, 3])
    w_mod = dt_in("w_mod", [D, 6 * D])
    b_mod = dt_in("b_mod", [1, 6 * D])
    w_in = dt_in("w_in", [D, 4480])
    convw_in = dt_in("convw", [128, 12, 9])
    pv_in = dt_in("pv", [128, NPV])
    w2_in = dt_in("w2", [128, 512])
    a2_in = dt_in("a2", [128, 512])
    g2_in = dt_in("g2", [128, 512])
    w_out = dt_in("w_out", [D, D])
    router_w = dt_in("router_w", [D, 64])
    router_b = dt_in("router_b", [1, 64])
    ewg = dt_in("ewg", [64, D, 256])
    ewu = dt_in("ewu", [64, D, 256])
    ewd = dt_in("ewd", [64, 256, D])
    swg = dt_in("swg", [D, 256])
    swu = dt_in("swu", [D, 256])
    swd = dt_in("swd", [256, D])
    fng = dt_in("fng", [1, D])
    ident_in = dt_in("ident", [128, 128])
    bdones_in = dt_in("bdones", [128, 128])
    masks_in = dt_in("masks", [2, 128, 512])
    out_d = nc.dram_tensor("out", [NB, SEQ, D], F32, kind="ExternalOutput").ap()
    mod_d = nc.dram_tensor("mod_d", [3, 6 * D], F32).ap()
    y_d = nc.dram_tensor("y_d", [NB, 8, 128, SEQ], BF16).ap()
    x1_d = nc.dram_tensor("x1_d", [NB, SEQ, D], F32).ap()
    dbg = {}
    for name, shape in DEBUG.items():
        dbg[name] = nc.dram_tensor("dbg_" + name, list(shape), F32, kind="ExternalOutput").ap()

    P = Prog(nc)
    V = lambda fn, r=(), w=(): P.op("vector", fn, r, w)
    S = lambda fn, r=(), w=(): P.op("scalar", fn, r, w)
    G = lambda fn, r=(), w=(): P.op("gpsimd", fn, r, w)
    TE = lambda fn, r=(), w=(): P.op("tensor", fn, r, w)

    def dump(name, src_ap, key):
        if name in dbg:
            P.dma(lambda e: e.dma_start(out=dbg[name], in_=src_ap), reads=[key], is_output=True, eng="gpsimd")

    w_in_v = w_in.rearrange("(p k) n -> p k n", k=8)

    with contextlib.ExitStack() as top:
        uid = [0]

        def SB(st, name, shape, dt=F32):
            uid[0] += 1
            return st.enter_context(nc.sbuf_tensor("%s_u%d" % (name, uid[0]), list(shape), dt))

        def PS(st, name, shape, dt=F32):
            uid[0] += 1
            return st.enter_context(nc.psum_tensor("%s_u%d" % (name, uid[0]), list(shape), dt))

        ident = SB(top, "ident", [128, 128])
        identb = SB(top, "identb", [128, 128], BF16)
        bdones = SB(top, "bdones", [128, 128])
        masks = SB(top, "masks", [128, 2, 512])
        pv = SB(top, "pv", [128, NPV])
        convw = SB(top, "convw", [128, 12, 9])
        w2b = SB(top, "w2b", [128, 512], BF16)
        a2b = SB(top, "a2b", [128, 512], BF16)
        g2b = SB(top, "g2b", [128, 512], BF16)
        rbbc = SB(top, "rbbc", [128, 64])
        modT = SB(top, "modT", [128, 3, 6, 8])
        gs1 = SB(top, "gs1", [128, 3, 8])
        gs2 = SB(top, "gs2", [128, 2, 8])
        lbv = SB(top, "lbv", [128, 2, 4, 3])
        P.dma(lambda e: e.dma_start(out=ident[:], in_=ident_in), writes=["ident"])
        P.dma(lambda e: e.dma_start(out=identb[:], in_=ident_in), writes=["identb"], eng="gpsimd")
        P.dma(lambda e: e.dma_start(out=bdones[:], in_=bdones_in), writes=["bdones"])
        for d in range(2):
            P.dma(lambda e, d=d: e.dma_start(out=masks[:, d, :], in_=masks_in[d]), writes=[("masks", d)])
        P.dma(lambda e: e.dma_start(out=pv[:], in_=pv_in), writes=["pv"])
        P.dma(lambda e: e.dma_start(out=convw[:], in_=convw_in), writes=["convw"])
        P.dma(lambda e: e.dma_start(out=w2b[:], in_=w2_in), writes=["w2b"], eng="gpsimd")
        P.dma(lambda e: e.dma_start(out=a2b[:], in_=a2_in), writes=["a2b"], eng="gpsimd")
        P.dma(lambda e: e.dma_start(out=g2b[:], in_=g2_in), writes=["g2b"], eng="gpsimd")
        P.dma(lambda e: e.dma_start(out=rbbc[:], in_=router_b.partition_broadcast(128)), writes=["rbbc"])
        pvc = lambda name, i=0, n=1: pv[:, PV[name] + i:PV[name] + i + n]
        V(lambda e: e.tensor_scalar(out=pvc("omka", 0, 4), in0=pvc("k_a", 0, 4), scalar1=-1.0, scalar2=1.0, op0=ALU.mult, op1=ALU.add), ["pv"], ["pv"])
        for d in range(2):
            lb0 = pv[:, PV["lbl"] + d * 8:PV["lbl"] + d * 8 + 4]
            lb1 = pv[:, PV["lbl"] + d * 8 + 4:PV["lbl"] + d * 8 + 8]
            V(lambda e, d=d, lb0=lb0, lb1=lb1: e.tensor_tensor(out=lbv[:, d, :, 0], in0=lb0, in1=lb1, op=ALU.subtract), ["pv"], ["lbv"])
            S(lambda e, d=d: e.activation(out=lbv[:, d, :, 0], in_=lbv[:, d, :, 0], func=AF.Sigmoid), ["lbv"], ["lbv"])
            V(lambda e, d=d: e.tensor_scalar(out=lbv[:, d, :, 1], in0=lbv[:, d, :, 0], scalar1=-1.0, scalar2=1.0, op0=ALU.mult, op1=ALU.add), ["lbv"], ["lbv"])
            V(lambda e, d=d: e.tensor_scalar(out=lbv[:, d, :, 2], in0=lbv[:, d, :, 1], scalar1=-1.0, scalar2=None, op0=ALU.mult), ["lbv"], ["lbv"])

        with contextlib.ExitStack() as st:
            cv = SB(st, "cv", [128, 8, 3])
            wm = [SB(st, "wm%d" % i, [128, 8, 512]) for i in range(4)]
            bm = [SB(st, "bm%d" % i, [3, 512]) for i in range(2)]
            mo = [SB(st, "mo%d" % i, [3, 512]) for i in range(2)]
            pm = [PS(st, "pm%d" % i, [128, 512]) for i in range(2)]
            P.dma(lambda e: e.dma_start(out=cv[:], in_=cvec_in), writes=["cv"])
            S(lambda e: e.activation(out=cv[:], in_=cv[:], func=AF.Silu), ["cv"], ["cv"])
            wmv = w_mod.rearrange("(p k) n -> p k n", k=8)
            import os
            for g in range(0 if os.environ.get("P0SKIP") else 12):
                i = g % 2
                wi_ = g % 4
                P.dma(lambda e, g=g, wi_=wi_: e.dma_start(out=wm[wi_][:], in_=wmv[:, :, g * 512:(g + 1) * 512]), writes=["wm%d" % wi_])
                P.dma(lambda e, g=g, i=i: e.dma_start(out=bm[i][:], in_=b_mod[:, g * 512:(g + 1) * 512].partition_broadcast(3)), writes=["bm%d" % i])
                for k in range(8):
                    TE(lambda e, i=i, k=k, wi_=wi_: e.matmul(pm[i][0:3, :], lhsT=cv[:, k, :], rhs=wm[wi_][:, k, :], start=(k == 0), stop=(k == 7)),
                       ["cv", "wm%d" % wi_], ["pm%d" % i])
                V(lambda e, i=i: e.tensor_tensor(out=mo[i][:], in0=pm[i][0:3, :], in1=bm[i][:], op=ALU.add), ["pm%d" % i, "bm%d" % i], ["mo%d" % i])
                P.dma(lambda e, g=g, i=i: e.dma_start(out=mod_d[:, g * 512:(g + 1) * 512], in_=mo[i][:]), reads=["mo%d" % i], writes=["mod_d"])
            for r in range(3):
                P.dma(lambda e, r=r: e.dma_start(out=modT[:, r], in_=mod_d[r].rearrange("(m p k) -> p m k", m=6, k=8)), reads=["mod_d"], writes=[("modT", r)])
            for r in range(3):
                V(lambda e, r=r: e.scalar_tensor_tensor(out=gs1[:, r, :], in0=modT[:, r, 1, :], scalar=1.0, in1=pvc("n1g", 0, 8), op0=ALU.add, op1=ALU.mult),
                  [("modT", r), "pv"], [("gs1", r)])
            for b in range(2):
                V(lambda e, b=b: e.scalar_tensor_tensor(out=gs2[:, b, :], in0=modT[:, b, 4, :], scalar=1.0, in1=pvc("n2g", 0, 8), op0=ALU.add, op1=ALU.mult),
                  [("modT", b), "pv"], [("gs2", b)])
        P.barrier()
        dump("modT", modT[:].rearrange("p a b c -> p (a b c)"), "modT")

        def rstd_from_ss(ss, key, eps_scale):
            V(lambda e: e.tensor_scalar(out=ss[:, 1:2], in0=ss[:, 0:1], scalar1=1.0 / D, scalar2=1e-6, op0=ALU.mult, op1=ALU.add), [key], [key])
            S(lambda e: e.activation(out=ss[:, 1:2], in_=ss[:, 1:2], func=AF.Sqrt), [key], [key])
            V(lambda e: e.reciprocal(out=ss[:, 1:2], in_=ss[:, 1:2]), [key], [key])

        def check(tag):
            if stop_after == tag and not getattr(P, "disabled", False):
                P.finish()
                with contextlib.ExitStack() as rs0:
                    P.replay(rs0)
                P.replayed = True
                P.disabled = True

        def _body():
            for b in range(NB):
                if stop_after == "mod":
                    break
                with contextlib.ExitStack() as sa:
                    hT = SB(sa, "hT", [128, 8, TT], BF16)
                    twl = SB(sa, "twl", [128, TT], BF16)
                    adl = SB(sa, "adl", [128, TT], BF16)
                    gsg = SB(sa, "gsg", [128, TT], BF16)
                    with contextlib.ExitStack() as st:
                        xt = [SB(st, "xt%d" % i, [128, D]) for i in range(2)]
                        junk = SB(st, "junk", [128, D])
                        ssq = [SB(st, "ssq%d" % i, [128, 2]) for i in range(2)]
                        pT = [PS(st, "pT%d" % i, [128, 4, 128]) for i in range(2)]
                        import os
                        for ti in range(int(os.environ.get("P1TILES", "18"))):
                            i = ti % 2
                            if ti < 2:
                                src = ctx_in[b, ti * 128:(ti + 1) * 128, :]
                                r = 2
                            else:
                                src = x_in[b, (ti - 2) * 128:(ti - 1) * 128, :]
                                r = b
                            P.dma(lambda e, i=i, src=src: e.dma_start(out=xt[i][:], in_=src), writes=["xt%d" % i])
                            G(lambda e, i=i: e.memset(ssq[i][:], 0.0), [], ["ssq%d" % i])
                            S(lambda e, i=i: e.activation(out=junk[:], in_=xt[i][:], func=AF.Square, accum_out=ssq[i][:, 0:1]), ["xt%d" % i], ["junk", "ssq%d" % i])
                            rstd_from_ss(ssq[i], "ssq%d" % i, None)
                            V(lambda e, i=i: e.tensor_scalar(out=xt[i][:], in0=xt[i][:], scalar1=ssq[i][:, 1:2], scalar2=None, op0=ALU.mult), ["xt%d" % i, "ssq%d" % i], ["xt%d" % i])
                            xv = xt[i][:].rearrange("t (p k) -> t k p", k=8)
                            for hb_ in range(2):
                                pb = pT[hb_]
                                for k in range(4 * hb_, 4 * hb_ + 4):
                                    TE(lambda e, k=k, pb=pb, xv=xv: e.matmul(pb[:, k % 4, :], lhsT=xv[:, k, :], rhs=ident[:], start=True, stop=True),
                                       ["xt%d" % i, "ident"], ["pT%d" % hb_])
                                for k in range(4 * hb_, 4 * hb_ + 4):
                                    S(lambda e, k=k, pb=pb, r=r, ti=ti: e.activation(out=hT[:, k, ti * 128:(ti + 1) * 128], in_=pb[:, k % 4, :], func=AF.Identity,
                                                                            scale=gs1[:, r, k:k + 1], bias=modT[:, r, 0, k:k + 1]),
                                      ["pT%d" % hb_, "gs1", "modT"], [("hT", ti)])
                    P.barrier()
                    if b == 0:
                        dump("hT", hT[:, 0, :], "hT")
                    check("p1")

                    ipc = [0]

                    def inproj(st_keys, wtile, wkey, wcol, evac, pA):
                        for ti, t0 in enumerate(range(0, TT, 512)):
                            n = min(512, TT - t0)
                            i = ipc[0] % 2
                            ipc[0] += 1
                            for k in range(8):
                                TE(lambda e, i=i, k=k, t0=t0, n=n: e.matmul(pA[i][:, 0:n], lhsT=wtile[:, k, wcol:wcol + 128], rhs=hT[:, k, t0:t0 + n],
                                                                      start=(k == 0), stop=(k == 7)), ["hT", wkey], ["pA%d" % i])
                            evac(pA[i], "pA%d" % i, t0, n)

                    with contextlib.ExitStack() as st:
                        wl = SB(st, "wl", [128, 8, 384], BF16)
                        pA = [PS(st, "pA%d" % i, [128, 512]) for i in range(2)]
                        P.dma(lambda e: e.dma_start(out=wl[:], in_=w_in_v[:, :, 1536:1920]), writes=["wl"], eng="gpsimd")
                        inproj(None, wl, "wl", 0, lambda ps, pk, t0, n: S(lambda e: e.activation(out=twl[:, t0:t0 + n], in_=ps[:, 0:n], func=AF.Tanh), [pk], ["twl"]), pA)
                        inproj(None, wl, "wl", 128, lambda ps, pk, t0, n: S(lambda e: e.activation(out=adl[:, t0:t0 + n], in_=ps[:, 0:n], func=AF.Identity), [pk], ["adl"]), pA)
                        inproj(None, wl, "wl", 256, lambda ps, pk, t0, n: S(lambda e: e.activation(out=gsg[:, t0:t0 + n], in_=ps[:, 0:n], func=AF.Sigmoid), [pk], ["gsg"]), pA)
                    P.barrier()

                    check("lora")
                    SEGS = [(0, 4)] + [(256 + 256 * i_, 4) for i_ in range(8)]
                    ORDER = {0: list(range(9)), 1: [0] + list(range(8, 0, -1))}

                    def decay_prep(tp, d, lw, lwkey, n, want_exc, sc=1.0):
                        nch = n // 64
                        gz = tp["gz"]
                        kx = lambda nm: nm + tp["sfx"]
                        V(lambda e: e.tensor_tensor_scan(out=gz[:, 1:1 + n], data0=tp["ones"][:, 0:n], data1=lw, initial=0.0, op0=ALU.mult, op1=ALU.add),
                          [lwkey, "ones"], [kx("gz")])
                        gv = lambda off: gz[:, off:off + n].rearrange("p (c j) -> p c j", j=64)
                        v3 = lambda t: t[:, 0:n].rearrange("p (c j) -> p c j", j=64)
                        if d == 0:
                            base = gv(0)[:, :, 0:1].to_broadcast([128, nch, 64])
                            V(lambda e: e.tensor_tensor(out=v3(tp["Dinc"]), in0=gv(1), in1=base, op=ALU.subtract), [kx("gz")], [kx("Dinc")])
                            if want_exc:
                                V(lambda e: e.tensor_tensor(out=v3(tp["Dexc"]), in0=gv(0), in1=base, op=ALU.subtract), [kx("gz")], [kx("Dexc")])
                        else:
                            top_ = gz[:, 64:64 + n].rearrange("p (c j) -> p c j", j=64)[:, :, 0:1].to_broadcast([128, nch, 64])
                            V(lambda e: e.tensor_tensor(out=v3(tp["Dinc"]), in0=top_, in1=gv(0), op=ALU.subtract), [kx("gz")], [kx("Dinc")])
                            if want_exc:
                                V(lambda e: e.tensor_tensor(out=v3(tp["Dexc"]), in0=top_, in1=gv(1), op=ALU.subtract), [kx("gz")], [kx("Dexc")])
                        S(lambda e: e.activation(out=tp["Eii"][:, 0:n], in_=tp["Dinc"][:, 0:n], func=AF.Exp, scale=-sc), [kx("Dinc")], [kx("Eii")])
                        S(lambda e: e.activation(out=tp["Dinc"][:, 0:n], in_=tp["Dinc"][:, 0:n], func=AF.Exp, scale=sc), [kx("Dinc")], [kx("Dinc")])
                        if want_exc:
                            S(lambda e: e.activation(out=tp["Dexc"][:, 0:n], in_=tp["Dexc"][:, 0:n], func=AF.Exp, scale=sc), [kx("Dexc")], [kx("Dexc")])

                    def rr_run(gens):
                        gens = list(gens)
                        while gens:
                            for g_ in list(gens):
                                try:
                                    next(g_)
                                except StopIteration:
                                    gens.remove(g_)

                    def run_unit(prep_pre, chain, order=None):
                        order = ORDER if order is None else order
                        ns = len(order[0])
                        rr_run([prep_pre(order[d][0], d, 0) for d in range(2)])
                        for i_ in range(ns):
                            gens = [chain(order[d][i_], d, i_ % 2) for d in range(2)]
                            if i_ + 1 < ns:
                                gens += [prep_pre(order[d][i_ + 1], d, (i_ + 1) % 2) for d in range(2)]
                            rr_run(gens)

                    SEGS_H = [(0, 4)] + [(256 + 512 * i_, 8) for i_ in range(4)]
                    ORDER_H = {0: list(range(5)), 1: [0] + list(range(4, 0, -1))}

                    def bd_write(eng, dst, dkey, col0, src, skeys, E, ekey, n, neg=False):
                        nch = n // 64
                        for u in range(2):
                            o = dst[u * 64:(u + 1) * 64, 0:nch, col0 + u * 64:col0 + (u + 1) * 64]
                            a = src[u * 64:(u + 1) * 64, :].rearrange("p (c j) -> p c j", j=64)
                            bb = E[u * 64:(u + 1) * 64, 0:n].rearrange("p (c j) -> p c j", j=64)
                            if neg:
                                P.op(eng, lambda e, o=o, a=a, bb=bb: e.scalar_tensor_tensor(out=o, in0=a, scalar=-1.0, in1=bb, op0=ALU.mult, op1=ALU.mult),
                                     list(skeys) + [ekey], [dkey])
                            else:
                                P.op(eng, lambda e, o=o, a=a, bb=bb: e.tensor_tensor(out=o, in0=a, in1=bb, op=ALU.mult), list(skeys) + [ekey], [dkey])

                    def h_update(d, bk, pHt, pHk, Hf, Hb, pc_ap, pckey):
                        HP = bk["HP"][d]
                        G(lambda e: e.tensor_scalar(out=HP[:], in0=Hf[:], scalar1=pc_ap, scalar2=None, op0=ALU.mult), ["Hf%d" % d, pckey], ["HP%d" % d])
                        V(lambda e: e.scalar_tensor_tensor(out=Hb[:], in0=pHt, scalar=pc_ap, in1=HP[:], op0=ALU.mult, op1=ALU.add), [pHk, pckey, "HP%d" % d], ["Hb%d" % d])
                        V(lambda e: e.scalar_tensor_tensor(out=Hf[:], in0=pHt, scalar=pc_ap, in1=HP[:], op0=ALU.mult, op1=ALU.add), [pHk, pckey, "HP%d" % d], ["Hf%d" % d])

                    def y_accum(d, first, pYt, pYk, Yacc, tok0):
                        dst = Yacc[:, tok0:tok0 + 64]
                        if True:
                            V(lambda e: e.tensor_tensor(out=dst, in0=dst, in1=pYt[:, 0:64], op=ALU.add), [pYk, ("Yacc", tok0)], [("Yacc", tok0)])
                            V(lambda e: e.tensor_tensor(out=dst, in0=dst, in1=pYt[:, 64:128], op=ALU.add), [pYk, ("Yacc", tok0)], [("Yacc", tok0)])

                    with contextlib.ExitStack() as su:
                        wu = SB(su, "wu", [128, 8, 384], BF16)
                        c0 = SB(su, "c0", [128, 2504], BF16)
                        dg = SB(su, "dg", [128, 9, 128], BF16)
                        rc = SB(su, "rc", [128, TT], BF16)
                        kc = SB(su, "kc", [128, TT], BF16)
                        vc = SB(su, "vc", [128, TT], BF16)
                        kk = SB(su, "kk", [128, TT], BF16)
                        kds = SB(su, "kds", [128, SEQ], BF16)
                        Yacc = SB(su, "Yacc", [128, SEQ])
                        tps = [{n_: SB(su, "tp%d_%s" % (d, n_), [128, 256]) for n_ in ("lw", "asg", "t1", "kd", "bb", "Dinc", "Dexc", "Eii")} for d in range(2)]
                        ones = SB(su, "tp_ones", [128, 256])
                        G(lambda e: e.memset(ones[:], 1.0), [], ["ones"])
                        for d in range(2):
                            tps[d]["gz"] = SB(su, "tp%d_gz" % d, [128, 384])
                            tps[d]["ones"] = ones
                            tps[d]["sfx"] = str(d)
                            G(lambda e, d=d: e.memset(tps[d]["gz"][:, 0:1], 0.0), [], ["gz%d" % d])
                        bk = {}
                        for nm, w_ in (("AR", 256), ("BT", 128), ("KT", 128), ("VT", 128), ("Bt", 128), ("Kt", 128), ("Vt", 128), ("nt2", 256), ("kt2", 256), ("mt", 128)):
                            bk[nm] = [[SB(su, "%s%d_%d" % (nm, d, s_), [128, 4, w_], BF16) for s_ in range(3 if nm in ("AR", "BT", "KT", "VT") else 2)] for d in range(2)]
                        bk["PC"] = [[SB(su, "PC%d_%d" % (d, s_), [128, 4]) for s_ in range(3)] for d in range(2)]
                        bk["Hf"] = [SB(su, "Hf%d" % d, [128, 128]) for d in range(2)]
                        bk["Hb"] = [SB(su, "Hb%d" % d, [128, 128], BF16) for d in range(2)]
                        bk["HP"] = [SB(su, "HP%d" % d, [128, 128]) for d in range(2)]
                        wkd = {nm: [SB(su, "%s_%d" % (nm, d), [128, 4, 128], BF16) for d in range(2)] for nm in ("nm", "xs0", "xs1", "xts0", "xts1")}
                        rhsb = [[SB(su, "rhs%d_%d" % (d, s_), [128, 128], BF16) for s_ in range(2)] for d in range(2)]
                        ubb = [[SB(su, "ub%d_%d" % (d, s_), [128, 128], BF16) for s_ in range(2)] for d in range(2)]
                        for d in range(2):
                            for s_ in range(3):
                                for nm in ("AR", "BT", "KT", "VT"):
                                    G(lambda e, nm=nm, d=d, s_=s_: e.memset(bk[nm][d][s_][:], 0.0), [], ["%s%d_p%d" % (nm, d, s_)])
                        print("RWKV scope sbuf remaining", nc.sbuf_bytes_remaining)
                        pA = [PS(su, "pA%d" % i, [128, 512]) for i in range(2)]
                        bC = [PS(su, "bC%d" % d, [128, 512]) for d in range(2)]
                        Wb = [PS(su, "bW%d" % i, [128, 512]) for i in range(4)]
                        pp = {"a": SB(su, "pp_a", [128, 512]), "b": SB(su, "pp_b", [128, 512])}

                        for p in range(4):
                            if stop_after == "h":
                                break
                            for j, col in enumerate((p * 128, 512 + p * 128, 1024 + p * 128)):
                                P.dma(lambda e, j=j, col=col: e.dma_start(out=wu[:, :, j * 128:(j + 1) * 128], in_=w_in_v[:, :, col:col + 128]), writes=["wu"], eng="gpsimd")
                            LAT0 = 259
                            G(lambda e: e.memset(c0[:], 0.0), [], ["c0"])
                            latv = c0[:, LAT0:LAT0 + 34 * 66].rearrange("p (y x) -> p y x", x=66)
                            idb9 = identb[:].rearrange("p (o x) -> p o x", o=1).to_broadcast([128, 9, 128])
                            for j, dstc in enumerate((rc, kc, vc)):
                                dk = ("rc", "kc", "vc")[j]
                                cwb = convw[:, j * 4 + p, :].rearrange("p (t o) -> p t o", o=1).to_broadcast([128, 9, 128])
                                V(lambda e, cwb=cwb: e.tensor_tensor(out=dg[:], in0=idb9, in1=cwb, op=ALU.mult), ["identb", "convw"], ["dg"])

                                def evac_pad(ps, pk, t0, n):
                                    if t0 == 0:
                                        S(lambda e: e.activation(out=c0[:, 1:1 + CTX], in_=ps[:, 0:CTX], func=AF.Identity), [pk], ["c0"])
                                        lo, r0 = CTX, 0
                                    else:
                                        lo, r0 = 0, (t0 - CTX) // 64
                                    nr = (n - lo) // 64
                                    S(lambda e: e.activation(out=latv[:, 1 + r0:1 + r0 + nr, 1:65], in_=ps[:, lo:n].rearrange("p (y x) -> p y x", x=64), func=AF.Identity), [pk], ["c0"])
                                inproj(None, wu, "wu", j * 128, evac_pad, pA)
                                wi_ = 0
                                wt, wtk = Wb[wi_ % 4], "bW%d" % (wi_ % 4)
                                for bb_ in range(3):
                                    TE(lambda e, bb_=bb_, wt=wt: e.matmul(wt[:, 0:CTX], lhsT=dg[:, 3 + bb_, :], rhs=c0[:, bb_:bb_ + CTX], start=(bb_ == 0), stop=(bb_ == 2)), ["dg", "c0"], [wtk])
                                S(lambda e, wt=wt, dstc=dstc: e.activation(out=dstc[:, 0:CTX], in_=wt[:, 0:CTX], func=AF.Identity), [wtk], [dk])
                                r0 = 0
                                while r0 < 32:
                                    nr = min(7, 32 - r0)
                                    wi_ += 1
                                    wt, wtk = Wb[wi_ % 4], "bW%d" % (wi_ % 4)
                                    nn = nr * 66
                                    ti_ = 0
                                    for a in range(3):
                                        for bb_ in range(3):
                                            off = LAT0 + (1 + r0 + a - 1) * 66 + (bb_ - 1)
                                            TE(lambda e, a=a, bb_=bb_, off=off, nn=nn, wt=wt, ti_=ti_: e.matmul(wt[:, 0:nn], lhsT=dg[:, a * 3 + bb_, :], rhs=c0[:, off:off + nn], start=(ti_ == 0), stop=(ti_ == 8)),
                                               ["dg", "c0"], [wtk])
                                            ti_ += 1
                                    src = wt[:, 0:nn].rearrange("p (y x) -> p y x", x=66)[:, :, 1:65]
                                    dst = dstc[:, CTX + r0 * 64:CTX + (r0 + nr) * 64].rearrange("p (y x) -> p y x", x=64)
                                    S(lambda e, src=src, dst=dst: e.activation(out=dst, in_=src, func=AF.Identity), [wtk], [dk])
                                    r0 += nr
                            if b == 0 and p == 0:
                                dump("rc", rc[:, :], "rc")
                                dump("vc", vc[:, :], "vc")
                            for t0 in range(0, TT, 512):
                                n = min(512, TT - t0)
                                i = (t0 // 512) % 2
                                V(lambda e, t0=t0, n=n: e.tensor_scalar(out=c0[:, t0:t0 + n], in0=kc[:, t0:t0 + n], scalar1=pvc("k_k", p), scalar2=None, op0=ALU.mult), ["kc", "pv"], [("c0", t0)])
                                G(lambda e, t0=t0, n=n: e.tensor_tensor(out=pp["a"][:, 0:n], in0=c0[:, t0:t0 + n], in1=c0[:, t0:t0 + n], op=ALU.mult), [("c0", t0)], ["pp_a"])
                                TE(lambda e, i=i, n=n: e.matmul(pA[i][:, 0:n], lhsT=bdones[:], rhs=pp["a"][:, 0:n], start=True, stop=True), ["bdones", "pp_a"], ["pA%d" % i])
                                S(lambda e, i=i, n=n: e.activation(out=pp["b"][:, 0:n], in_=pA[i][:, 0:n], func=AF.Sqrt), ["pA%d" % i], ["pp_b"])
                                V(lambda e, n=n: e.tensor_scalar(out=pp["b"][:, 0:n], in0=pp["b"][:, 0:n], scalar1=1e-12, scalar2=None, op0=ALU.max), ["pp_b"], ["pp_b"])
                                V(lambda e, n=n: e.reciprocal(out=pp["b"][:, 0:n], in_=pp["b"][:, 0:n]), ["pp_b"], ["pp_b"])
                                V(lambda e, t0=t0, n=n: e.tensor_tensor(out=kk[:, t0:t0 + n], in0=c0[:, t0:t0 + n], in1=pp["b"][:, 0:n], op=ALU.mult), [("c0", t0), "pp_b"], [("kk", t0)])
                            if b == 0 and p == 0:
                                dump("kk", kk[:, :], "kk")
                            check("kk")

                            def rw_prep(si, d, slot, pslot):
                                tp = tps[d]
                                sf = "%d_%d" % (d, slot)
                                K_ = lambda nm: (nm + "%d_p%d" % (d, pslot)) if nm in ("AR", "BT", "KT", "VT", "PC") else (nm + sf)
                                tk = lambda nm: nm + str(d)
                                t0, nch = SEGS[si]
                                n = nch * 64
                                sl = slice(t0, t0 + n)
                                dd = slice(d * 64, (d + 1) * 64)
                                fcol = slice(p * 128, (p + 1) * 128)
                                pa, pak = pA[d], "pA%d" % d
                                AR, BT, KT, VT = (bk[nm][d][pslot] for nm in ("AR", "BT", "KT", "VT"))
                                Bt, Kt, Vt = (bk[nm][d][slot] for nm in ("Bt", "Kt", "Vt"))
                                nt2, kt2, mt = (bk[nm][d][slot] for nm in ("nt2", "kt2", "mt"))
                                Wa, Wc = Wb[2 * d], Wb[2 * d + 1]
                                Wak, Wck = "bW%d" % (2 * d), "bW%d" % (2 * d + 1)
                                TE(lambda e: e.matmul(pa[:, 0:n], lhsT=w2b[dd, fcol], rhs=twl[dd, sl], start=True, stop=True), ["w2b", "twl"], [pak])
                                TE(lambda e: e.matmul(pa[:, 256:256 + n], lhsT=a2b[dd, fcol], rhs=adl[dd, sl], start=True, stop=True), ["a2b", "adl"], [pak])
                                S(lambda e: e.activation(out=tp["lw"][:, 0:n], in_=pa[:, 0:n], func=AF.Sigmoid, bias=pvc("w0", d * 4 + p)), [pak, "pv"], [tk("lw")])
                                S(lambda e: e.activation(out=tp["asg"][:, 0:n], in_=pa[:, 256:256 + n], func=AF.Sigmoid, bias=pvc("a0", d * 4 + p)), [pak, "pv"], [tk("asg")])
                                yield
                                decay_prep(tp, d, tp["lw"][:, 0:n], tk("lw"), n, True, sc=-LAM)
                                yield
                                S(lambda e: e.activation(out=tp["t1"][:, 0:n], in_=tp["asg"][:, 0:n], func=AF.Identity, scale=pvc("k_a", p), bias=pvc("omka", p)), [tk("asg"), "pv"], [tk("t1")])
                                G(lambda e: e.tensor_tensor(out=tp["kd"][:, 0:n], in0=tp["t1"][:, 0:n], in1=kc[:, sl], op=ALU.mult), [tk("t1"), "kc"], [tk("kd")])
                                V(lambda e: e.tensor_tensor(out=tp["bb"][:, 0:n], in0=tp["asg"][:, 0:n], in1=kk[:, sl], op=ALU.mult), [tk("asg"), "kk"], [tk("bb")])
                                if si > 0:
                                    ks = kds[:, t0 - CTX:t0 - CTX + n]
                                    G(lambda e: e.tensor_tensor(out=ks, in0=ks, in1=tp["kd"][:, 0:n], op=ALU.add), [tk("kd"), ("kds", si)], [("kds", si)])
                                yield
                                bd_write("vector", AR, K_("AR"), 0, kk[:, sl], ["kk"], tp["Dexc"], tk("Dexc"), n, neg=True)
                                bd_write("gpsimd", AR, K_("AR"), 128, rc[:, sl], ["rc"], tp["Dinc"], tk("Dinc"), n)
                                bd_write("vector", BT, K_("BT"), 0, tp["bb"][:, 0:n], [tk("bb")], tp["Eii"], tk("Eii"), n)
                                bd_write("vector", KT, K_("KT"), 0, tp["kd"][:, 0:n], [tk("kd")], tp["Eii"], tk("Eii"), n)
                                pcsrc = tp["Dinc"][:, 0:n].rearrange("p (c j) -> p c j", j=64)[:, :, (63 if d == 0 else 0)]
                                V(lambda e: e.tensor_copy(out=bk["PC"][d][pslot][:, 0:nch], in_=pcsrc), [tk("Dinc")], [K_("PC")])
                                for u in range(2):
                                    o = VT[u * 64:(u + 1) * 64, 0:nch, u * 64:(u + 1) * 64]
                                    a_ = vc[u * 64:(u + 1) * 64, sl].rearrange("p (c j) -> p c j", j=64)
                                    S(lambda e, o=o, a_=a_: e.activation(out=o, in_=a_, func=AF.Identity), ["vc"], [K_("VT")])
                                yield

                            def rw_pre(si, d, slot, pslot):
                                tp = tps[d]
                                sf = "%d_%d" % (d, slot)
                                K_ = lambda nm: (nm + "%d_p%d" % (d, pslot)) if nm in ("AR", "BT", "KT", "VT", "PC") else (nm + sf)
                                tk = lambda nm: nm + str(d)
                                t0, nch = SEGS[si]
                                n = nch * 64
                                sl = slice(t0, t0 + n)
                                dd = slice(d * 64, (d + 1) * 64)
                                fcol = slice(p * 128, (p + 1) * 128)
                                pa, pak = pA[d], "pA%d" % d
                                AR, BT, KT, VT = (bk[nm][d][pslot] for nm in ("AR", "BT", "KT", "VT"))
                                Bt, Kt, Vt = (bk[nm][d][slot] for nm in ("Bt", "Kt", "Vt"))
                                nt2, kt2, mt = (bk[nm][d][slot] for nm in ("nt2", "kt2", "mt"))
                                Wa, Wc = Wb[2 * d], Wb[2 * d + 1]
                                Wak, Wck = "bW%d" % (2 * d), "bW%d" % (2 * d + 1)
                                m2b = masks[:, d:d + 1, 0:256].to_broadcast([128, 2, 256])
                                m1b = masks[:, d:d + 1, 256:384].to_broadcast([128, 4, 128])
                                idb4 = identb[:].rearrange("p (o x) -> p o x", o=1).to_broadcast([128, 4, 128])
                                w2v = lambda wt: wt[:].rearrange("p (c x) -> p c x", x=256)
                                w4v = lambda wt: wt[:].rearrange("p (c x) -> p c x", x=128)
                                for lhs_, lk, dst, dk in ((BT, K_("BT"), nt2, K_("nt2")), (KT, K_("KT"), kt2, K_("kt2"))):
                                    for j in range(4):
                                        wt, wtk = (Wa, Wak) if j < 2 else (Wc, Wck)
                                        TE(lambda e, j=j, wt=wt, lhs_=lhs_: e.matmul(wt[:, (j % 2) * 256:(j % 2 + 1) * 256], lhsT=lhs_[:, j, :], rhs=AR[:, j, :], start=True, stop=True), [lk, K_("AR")], [wtk])
                                    V(lambda e, dst=dst: e.tensor_tensor(out=dst[:, 0:2, :], in0=w2v(Wa), in1=m2b, op=ALU.mult), [Wak, "masks"], [dk])
                                    V(lambda e, dst=dst: e.tensor_tensor(out=dst[:, 2:4, :], in0=w2v(Wc), in1=m2b, op=ALU.mult), [Wck, "masks"], [dk])
                                    yield
                                nm_ = wkd["nm"][d]
                                for j in range(4):
                                    TE(lambda e, j=j: e.matmul(Wa[:, j * 128:(j + 1) * 128], lhsT=AR[:, j, 0:128], rhs=BT[:, j, :], start=True, stop=True), [K_("AR"), K_("BT")], [Wak])
                                for j in range(4):
                                    TE(lambda e, j=j: e.matmul(Wc[:, j * 128:(j + 1) * 128], lhsT=BT[:, j, :], rhs=identb[:], start=True, stop=True), [K_("BT"), "identb"], [Wck])
                                V(lambda e: e.tensor_tensor(out=nm_[:], in0=w4v(Wa), in1=m1b, op=ALU.mult), [Wak, "masks"], [tk("nm")])
                                S(lambda e: e.activation(out=Bt[:], in_=w4v(Wc), func=AF.Identity), [Wck], [K_("Bt")])
                                yield
                                for j in range(4):
                                    TE(lambda e, j=j: e.matmul(Wa[:, j * 128:(j + 1) * 128], lhsT=KT[:, j, :], rhs=identb[:], start=True, stop=True), [K_("KT"), "identb"], [Wak])
                                for j in range(4):
                                    TE(lambda e, j=j: e.matmul(Wc[:, j * 128:(j + 1) * 128], lhsT=VT[:, j, :], rhs=identb[:], start=True, stop=True), [K_("VT"), "identb"], [Wck])
                                S(lambda e: e.activation(out=Kt[:], in_=w4v(Wa), func=AF.Identity), [Wak], [K_("Kt")])
                                V(lambda e: e.tensor_copy(out=Vt[:], in_=w4v(Wc)), [Wck], [K_("Vt")])
                                V(lambda e: e.tensor_tensor(out=mt[:], in0=nt2[:, :, 0:128], in1=idb4, op=ALU.add), [K_("nt2"), "identb"], [K_("mt")])
                                yield
                                X, XT, Xk, XTk = nm_, nt2[:, :, 0:128], tk("nm"), K_("nt2")
                                import os
                                for j in range(1, 1 if os.environ.get("RWSKIP") == "inv" else 6):
                                    xn_, xtn_ = wkd["xs%d" % (j % 2)][d], wkd["xts%d" % (j % 2)][d]
                                    xnk, xtnk = tk("xs%d" % (j % 2)), tk("xts%d" % (j % 2))
                                    for c in range(4):
                                        TE(lambda e, c=c, X=X, XT=XT: e.matmul(Wa[:, c * 128:(c + 1) * 128], lhsT=XT[:, c, :], rhs=X[:, c, :], start=True, stop=True), [Xk, XTk], [Wak])
                                    if j < 5:
                                        for c in range(4):
                                            TE(lambda e, c=c, X=X, XT=XT: e.matmul(Wc[:, c * 128:(c + 1) * 128], lhsT=X[:, c, :], rhs=XT[:, c, :], start=True, stop=True), [Xk, XTk], [Wck])
                                    S(lambda e, xn_=xn_: e.activation(out=xn_[:], in_=w4v(Wa), func=AF.Identity), [Wak], [xnk])
                                    if j < 5:
                                        S(lambda e, xtn_=xtn_: e.activation(out=xtn_[:], in_=w4v(Wc), func=AF.Identity), [Wck], [xtnk])
                                    yield
                                    for c in range(4):
                                        TE(lambda e, c=c: e.matmul(Wa[:, c * 128:(c + 1) * 128], lhsT=identb[:], rhs=mt[:, c, :], start=True, stop=False), ["identb", K_("mt")], [Wak])
                                        TE(lambda e, c=c, xn_=xn_: e.matmul(Wa[:, c * 128:(c + 1) * 128], lhsT=xn_[:, c, :], rhs=mt[:, c, :], start=False, stop=True), [xnk, K_("mt")], [Wak])
                                    S(lambda e: e.activation(out=mt[:], in_=w4v(Wa), func=AF.Identity), [Wak, K_("mt")], [K_("mt")])
                                    X, XT, Xk, XTk = xn_[:], xtn_[:], xnk, xtnk
                                    yield

                            def rw_chain(si, d, slot, pslot):
                                sf = "%d_%d" % (d, slot)
                                K_ = lambda nm: (nm + "%d_p%d" % (d, pslot)) if nm in ("AR", "BT", "KT", "VT", "PC") else (nm + sf)
                                t0, nch = SEGS[si]
                                AR = bk["AR"][d][pslot]
                                Bt, Kt, Vt = (bk[nm][d][slot] for nm in ("Bt", "Kt", "Vt"))
                                nt2, kt2, mt = (bk[nm][d][slot] for nm in ("nt2", "kt2", "mt"))
                                Hf, Hb = bk["Hf"][d], bk["Hb"][d]
                                bCk = "bC%d" % d
                                order = list(range(nch)) if d == 0 else list(range(nch))[::-1]
                                import os
                                if os.environ.get("RWSKIP") == "chain":
                                    return
                                for ci, c in enumerate(order):
                                    rb, ub = rhsb[d][ci % 2], ubb[d][ci % 2]
                                    rbk, ubk = "rhs%d_%d" % (d, ci % 2), "ub%d_%d" % (d, ci % 2)
                                    TE(lambda e, c=c: e.matmul(bC[d][:, 0:128], lhsT=AR[:, c, 0:128], rhs=Hb[:], start=True, stop=False), [K_("AR"), "Hb%d" % d], [bCk])
                                    TE(lambda e, c=c: e.matmul(bC[d][:, 0:128], lhsT=kt2[:, c, 0:128], rhs=Vt[:, c, :], start=False, stop=True), [K_("kt2"), K_("Vt")], [bCk])
                                    S(lambda e, rb=rb: e.activation(out=rb[:], in_=bC[d][:, 0:128], func=AF.Identity), [bCk], [rbk])
                                    yield
                                    TE(lambda e, c=c, rb=rb: e.matmul(bC[d][:, 128:256], lhsT=mt[:, c, :], rhs=rb[:], start=True, stop=True), [K_("mt"), rbk], [bCk])
                                    S(lambda e, ub=ub: e.activation(out=ub[:], in_=bC[d][:, 128:256], func=AF.Identity), [bCk], [ubk])
                                    yield
                                    if si > 0:
                                        TE(lambda e, c=c: e.matmul(bC[d][:, 256:384], lhsT=Hb[:], rhs=AR[:, c, 128:256], start=True, stop=False), [K_("AR"), "Hb%d" % d], [bCk])
                                        TE(lambda e, c=c, ub=ub: e.matmul(bC[d][:, 256:384], lhsT=ub[:], rhs=nt2[:, c, 128:256], start=False, stop=False), [ubk, K_("nt2")], [bCk])
                                        TE(lambda e, c=c: e.matmul(bC[d][:, 256:384], lhsT=Vt[:, c, :], rhs=kt2[:, c, 128:256], start=False, stop=True), [K_("Vt"), K_("kt2")], [bCk])
                                    TE(lambda e, c=c, ub=ub: e.matmul(bC[d][:, 384:512], lhsT=Bt[:, c, :], rhs=ub[:], start=True, stop=False), [K_("Bt"), ubk], [bCk])
                                    TE(lambda e, c=c: e.matmul(bC[d][:, 384:512], lhsT=Kt[:, c, :], rhs=Vt[:, c, :], start=False, stop=True), [K_("Kt"), K_("Vt")], [bCk])
                                    h_update(d, bk, bC[d][:, 384:512], bCk, Hf, Hb, bk["PC"][d][pslot][:, c:c + 1], K_("PC"))
                                    if si > 0:
                                        y_accum(d, False, bC[d][:, 256:384], bCk, Yacc, t0 - CTX + c * 64)
                                    yield

                            G(lambda e: e.memset(kds[:], 0.0), [], ["kds"])
                            G(lambda e: e.memset(Yacc[:], 0.0), [], ["Yacc"])
                            for d in range(2):
                                G(lambda e, d=d: e.memset(bk["Hf"][d][:], 0.0), [], ["Hf%d" % d])
                                G(lambda e, d=d: e.memset(bk["Hb"][d][:], 0.0), [], ["Hb%d" % d])
                            ns_ = len(ORDER[0])
                            rr_run([rw_prep(ORDER[d][0], d, 0, 0) for d in range(2)])
                            rr_run([rw_pre(ORDER[d][0], d, 0, 0) for d in range(2)] + [rw_prep(ORDER[d][1], d, 0, 1) for d in range(2)])
                            for i_ in range(ns_):
                                gens = [rw_chain(ORDER[d][i_], d, i_ % 2, i_ % 3) for d in range(2)]
                                if i_ + 1 < ns_:
                                    gens += [rw_pre(ORDER[d][i_ + 1], d, (i_ + 1) % 2, (i_ + 1) % 3) for d in range(2)]
                                if i_ + 2 < ns_:
                                    gens += [rw_prep(ORDER[d][i_ + 2], d, 0, (i_ + 2) % 3) for d in range(2)]
                                rr_run(gens)
                            if b == 0 and p == 0:
                                dump("Yacc", Yacc[:, :], "Yacc")
                            check("chain")
                            A_, B_ = pp["a"], pp["b"]
                            for t0 in range(0, SEQ, 512):
                                i = (t0 // 512) % 2
                                sl = slice(t0, t0 + 512)
                                cs = slice(CTX + t0, CTX + t0 + 512)
                                wbn, wbnk = Wb[i], "bW%d" % i
                                wgt, wgtk = Wb[2 + i], "bW%d" % (2 + i)
                                V(lambda e, sl=sl, cs=cs: e.scalar_tensor_tensor(out=B_[:, 0:512], in0=rc[:, cs], scalar=pvc("r_k", p), in1=kds[:, sl], op0=ALU.mult, op1=ALU.mult), ["rc", "kds", "pv"], ["pp_b"])
                                TE(lambda e, wbn=wbn: e.matmul(wbn[:], lhsT=bdones[:], rhs=B_[:, 0:512], start=True, stop=True), ["bdones", "pp_b"], [wbnk])
                                TE(lambda e, wgt=wgt, cs=cs: e.matmul(wgt[:], lhsT=g2b[:, p * 128:(p + 1) * 128], rhs=gsg[:, cs], start=True, stop=True), ["g2b", "gsg"], [wgtk])
                                TE(lambda e, i=i, sl=sl: e.matmul(pA[i][:], lhsT=bdones[:], rhs=Yacc[:, sl], start=True, stop=True), ["bdones", "Yacc"], ["pA%d" % i])
                                V(lambda e, i=i, sl=sl: e.scalar_tensor_tensor(out=Yacc[:, sl], in0=pA[i][:], scalar=-1.0 / 64, in1=Yacc[:, sl], op0=ALU.mult, op1=ALU.add), ["pA%d" % i, "Yacc"], ["Yacc"])
                                V(lambda e, sl=sl: e.tensor_tensor(out=A_[:, 0:512], in0=Yacc[:, sl], in1=Yacc[:, sl], op=ALU.mult), ["Yacc"], ["pp_a"])
                                TE(lambda e, i=i, sl=sl: e.matmul(pA[i][:], lhsT=bdones[:], rhs=A_[:, 0:512], start=True, stop=True), ["bdones", "pp_a"], ["pA%d" % i])
                                V(lambda e, i=i, sl=sl: e.tensor_scalar(out=A_[:, 0:512], in0=pA[i][:], scalar1=1.0 / 64, scalar2=64e-5, op0=ALU.mult, op1=ALU.add), ["pA%d" % i], ["pp_a"])
                                S(lambda e, sl=sl: e.activation(out=A_[:, 0:512], in_=A_[:, 0:512], func=AF.Sqrt), ["pp_a"], ["pp_a"])
                                V(lambda e, sl=sl: e.reciprocal(out=A_[:, 0:512], in_=A_[:, 0:512]), ["pp_a"], ["pp_a"])
                                V(lambda e, sl=sl: e.tensor_tensor(out=Yacc[:, sl], in0=Yacc[:, sl], in1=A_[:, 0:512], op=ALU.mult), ["Yacc", "pp_a"], ["Yacc"])
                                V(lambda e, sl=sl: e.tensor_scalar(out=Yacc[:, sl], in0=Yacc[:, sl], scalar1=pvc("lnx_g", p), scalar2=pvc("lnx_b", p), op0=ALU.mult, op1=ALU.add), ["Yacc", "pv"], ["Yacc"])
                                V(lambda e, wbn=wbn, cs=cs: e.tensor_tensor(out=B_[:, 0:512], in0=wbn[:], in1=vc[:, cs], op=ALU.mult), [wbnk, "vc"], ["pp_b"])
                                V(lambda e, sl=sl: e.tensor_tensor(out=Yacc[:, sl], in0=Yacc[:, sl], in1=B_[:, 0:512], op=ALU.add), ["Yacc", "pp_b"], ["Yacc"])
                                V(lambda e, wgt=wgt, sl=sl: e.tensor_tensor(out=c0[:, sl], in0=wgt[:], in1=Yacc[:, sl], op=ALU.mult), [wgtk, "Yacc"], ["c0"])
                            P.dma(lambda e, p=p: e.dma_start(out=y_d[b, p], in_=c0[:, 0:SEQ]), reads=["c0"], writes=[("y_d", b * 8 + p)])
                    P.barrier()
                    check("rwall")

                    with contextlib.ExitStack() as su:
                        wu = SB(su, "wu", [128, 8, 640], BF16)
                        qs = SB(su, "qs", [128, TT], BF16)
                        vs = SB(su, "vs", [128, TT], BF16)
                        gg = SB(su, "gg", [128, TT], BF16)
                        fsg = [SB(su, "fsg%d" % d, [128, TT]) for d in range(2)]
                        Yacc = SB(su, "Yacc", [128, SEQ])
                        tps = [{n_: SB(su, "tp%d_%s" % (d, n_), [128, 512]) for n_ in ("lw", "kf", "Dinc", "Eii")} for d in range(2)]
                        ones = SB(su, "tp_ones", [128, 512])
                        G(lambda e: e.memset(ones[:], 1.0), [], ["ones"])
                        for d in range(2):
                            tps[d]["gz"] = SB(su, "tp%d_gz" % d, [128, 640])
                            tps[d]["ones"] = ones
                            tps[d]["sfx"] = str(d)
                            G(lambda e, d=d: e.memset(tps[d]["gz"][:, 0:1], 0.0), [], ["gz%d" % d])
                        bk = {}
                        for nm in ("QT", "KT", "VT", "Kt", "Vt", "st"):
                            bk[nm] = [[SB(su, "%s%d_%d" % (nm, d, s_), [128, 8, 128], BF16) for s_ in range(2)] for d in range(2)]
                        bk["PC"] = [[SB(su, "PC%d_%d" % (d, s_), [128, 8]) for s_ in range(2)] for d in range(2)]
                        KHs = [SB(su, "KH%d" % d, [128, 8, 128], BF16) for d in range(2)]
                        bk["Hf"] = [SB(su, "Hf%d" % d, [128, 128]) for d in range(2)]
                        bk["Hb"] = [SB(su, "Hb%d" % d, [128, 128], BF16) for d in range(2)]
                        bk["HP"] = [SB(su, "HP%d" % d, [128, 128]) for d in range(2)]
                        for d in range(2):
                            for s_ in range(2):
                                for nm in ("QT", "KT", "VT"):
                                    G(lambda e, nm=nm, d=d, s_=s_: e.memset(bk[nm][d][s_][:], 0.0), [], ["%s%d_%d" % (nm, d, s_)])
                        pA = [PS(su, "pA%d" % i, [128, 512]) for i in range(2)]
                        bC = [PS(su, "bC%d" % d, [128, 512]) for d in range(2)]
                        Wb = [PS(su, "bW%d" % i, [128, 512]) for i in range(4)]
                        ystage = SB(su, "ystage", [128, SEQ], BF16)
                        A_ = SB(su, "pp_a", [128, SEQ])
                        for p in range(4):
                            for j, col in enumerate((1920, 2432, 2944, 3456, 3968)):
                                P.dma(lambda e, j=j, col=col: e.dma_start(out=wu[:, :, j * 128:(j + 1) * 128], in_=w_in_v[:, :, col + p * 128:col + (p + 1) * 128]), writes=["wu"], eng="gpsimd")
                            inproj(None, wu, "wu", 0, lambda ps, pk, t0, n: S(lambda e: e.activation(out=A_[:, 0:n], in_=ps[:, 0:n], func=AF.Silu), [pk], ["pp_a"]) and
                                   V(lambda e: e.tensor_scalar(out=qs[:, t0:t0 + n], in0=A_[:, 0:n], scalar1=0.125, scalar2=None, op0=ALU.mult), ["pp_a"], ["qs"]), pA)
                            inproj(None, wu, "wu", 128, lambda ps, pk, t0, n: S(lambda e: e.activation(out=fsg[0][:, t0:t0 + n], in_=ps[:, 0:n], func=AF.Sigmoid), [pk], ["fsg0"]), pA)
                            inproj(None, wu, "wu", 256, lambda ps, pk, t0, n: S(lambda e: e.activation(out=fsg[1][:, t0:t0 + n], in_=ps[:, 0:n], func=AF.Sigmoid), [pk], ["fsg1"]), pA)
                            inproj(None, wu, "wu", 384, lambda ps, pk, t0, n: S(lambda e: e.activation(out=vs[:, t0:t0 + n], in_=ps[:, 0:n], func=AF.Identity), [pk], ["vs"]), pA)
                            inproj(None, wu, "wu", 512, lambda ps, pk, t0, n: S(lambda e: e.activation(out=gg[:, t0:t0 + n], in_=ps[:, 0:n], func=AF.Silu), [pk], ["gg"]), pA)

                            check("hgin")
                            def hg_prep_pre(si, d, slot):
                                tp = tps[d]
                                sf = "%d_%d" % (d, slot)
                                K_ = lambda nm: nm + sf
                                tk = lambda nm: nm + str(d)
                                t0, nch = SEGS_H[si]
                                n = nch * 64
                                sl = slice(t0, t0 + n)
                                QT, KT, VT, Kt, Vt, st_ = (bk[nm][d][slot] for nm in ("QT", "KT", "VT", "Kt", "Vt", "st"))
                                Wa, Wc = Wb[2 * d], Wb[2 * d + 1]
                                Wak, Wck = "bW%d" % (2 * d), "bW%d" % (2 * d + 1)
                                V(lambda e: e.tensor_scalar(out=tp["lw"][:, 0:n], in0=fsg[d][:, sl], scalar1=lbv[:, d, p, 1:2], scalar2=lbv[:, d, p, 0:1], op0=ALU.mult, op1=ALU.add), ["fsg%d" % d, "lbv"], [tk("lw")])
                                S(lambda e: e.activation(out=tp["lw"][:, 0:n], in_=tp["lw"][:, 0:n], func=AF.Ln), [tk("lw")], [tk("lw")])
                                S(lambda e: e.activation(out=tp["kf"][:, 0:n], in_=fsg[d][:, sl], func=AF.Identity, scale=lbv[:, d, p, 2:3], bias=lbv[:, d, p, 1:2]), ["fsg%d" % d, "lbv"], [tk("kf")])
                                yield
                                decay_prep(tp, d, tp["lw"][:, 0:n], tk("lw"), n, False)
                                yield
                                bd_write("vector", QT, K_("QT"), 0, qs[:, sl], ["qs"], tp["Dinc"], tk("Dinc"), n)
                                bd_write("gpsimd", KT, K_("KT"), 0, tp["kf"][:, 0:n], [tk("kf")], tp["Eii"], tk("Eii"), n)
                                pcsrc = tp["Dinc"][:, 0:n].rearrange("p (c j) -> p c j", j=64)[:, :, (63 if d == 0 else 0)]
                                V(lambda e: e.tensor_copy(out=bk["PC"][d][slot][:, 0:nch], in_=pcsrc), [tk("Dinc")], [K_("PC")])
                                KH = KHs[d]
                                pcb = bk["PC"][d][slot][:, 0:nch].rearrange("p (c o) -> p c o", o=1).to_broadcast([128, nch, 128])
                                V(lambda e: e.tensor_tensor(out=KH[:, 0:nch, :], in0=KT[:, 0:nch, :], in1=pcb, op=ALU.mult), [K_("KT"), K_("PC")], [tk("KH")])
                                for u in range(2):
                                    o = VT[u * 64:(u + 1) * 64, 0:nch, u * 64:(u + 1) * 64]
                                    a_ = vs[u * 64:(u + 1) * 64, sl].rearrange("p (c j) -> p c j", j=64)
                                    S(lambda e, o=o, a_=a_: e.activation(out=o, in_=a_, func=AF.Identity), ["vs"], [K_("VT")])
                                yield
                                mIb = masks[:, d:d + 1, 128:256].to_broadcast([128, 4, 128])
                                w4v = lambda wt: wt[:].rearrange("p (c x) -> p c x", x=128)
                                halves = [(0, Wa, Wak)] + ([(4, Wc, Wck)] if nch > 4 else [])
                                for c0_, wt, wtk in halves:
                                    for j in range(4):
                                        TE(lambda e, j=j, c0_=c0_, wt=wt: e.matmul(wt[:, j * 128:(j + 1) * 128], lhsT=KT[:, c0_ + j, :], rhs=QT[:, c0_ + j, :], start=True, stop=True), [K_("KT"), K_("QT")], [wtk])
                                for c0_, wt, wtk in halves:
                                    V(lambda e, c0_=c0_, wt=wt: e.tensor_tensor(out=st_[:, c0_:c0_ + 4, :], in0=w4v(wt), in1=mIb, op=ALU.mult), [wtk, "masks"], [K_("st")])
                                yield
                                for c0_, wt, wtk in halves:
                                    for j in range(4):
                                        TE(lambda e, j=j, c0_=c0_, wt=wt: e.matmul(wt[:, j * 128:(j + 1) * 128], lhsT=KH[:, c0_ + j, :], rhs=identb[:], start=True, stop=True), [tk("KH"), "identb"], [wtk])
                                for c0_, wt, wtk in halves:
                                    S(lambda e, c0_=c0_, wt=wt: e.activation(out=Kt[:, c0_:c0_ + 4, :], in_=w4v(wt), func=AF.Identity), [wtk], [K_("Kt")])
                                yield
                                for c0_, wt, wtk in halves:
                                    for j in range(4):
                                        TE(lambda e, j=j, c0_=c0_, wt=wt: e.matmul(wt[:, j * 128:(j + 1) * 128], lhsT=VT[:, c0_ + j, :], rhs=identb[:], start=True, stop=True), [K_("VT"), "identb"], [wtk])
                                for i_h, (c0_, wt, wtk) in enumerate(halves):
                                    if i_h == 0:
                                        S(lambda e, c0_=c0_, wt=wt: e.activation(out=Vt[:, c0_:c0_ + 4, :], in_=w4v(wt), func=AF.Identity), [wtk], [K_("Vt")])
                                    else:
                                        V(lambda e, c0_=c0_, wt=wt: e.tensor_copy(out=Vt[:, c0_:c0_ + 4, :], in_=w4v(wt)), [wtk], [K_("Vt")])
                                yield

                            def hg_chain(si, d, slot):
                                sf = "%d_%d" % (d, slot)
                                K_ = lambda nm: nm + sf
                                t0, nch = SEGS_H[si]
                                QT, Kt, Vt, st_ = (bk[nm][d][slot] for nm in ("QT", "Kt", "Vt", "st"))
                                Hf, Hb = bk["Hf"][d], bk["Hb"][d]
                                bCk = "bC%d" % d
                                order = list(range(nch)) if d == 0 else list(range(nch))[::-1]
                                for c in order:
                                    if si > 0:
                                        TE(lambda e, c=c: e.matmul(bC[d][:, 0:128], lhsT=Vt[:, c, :], rhs=st_[:, c, :], start=True, stop=False), [K_("Vt"), K_("st")], [bCk])
                                        TE(lambda e, c=c: e.matmul(bC[d][:, 0:128], lhsT=Hb[:], rhs=QT[:, c, :], start=False, stop=True), ["Hb%d" % d, K_("QT")], [bCk])
                                    TE(lambda e, c=c: e.matmul(bC[d][:, 128:256], lhsT=Kt[:, c, :], rhs=Vt[:, c, :], start=True, stop=True), [K_("Kt"), K_("Vt")], [bCk])
                                    pc_ = bk["PC"][d][slot][:, c:c + 1]
                                    V(lambda e, pc_=pc_: e.scalar_tensor_tensor(out=Hb[:], in0=Hf[:], scalar=pc_, in1=bC[d][:, 128:256], op0=ALU.mult, op1=ALU.add), ["Hf%d" % d, K_("PC"), bCk], ["Hb%d" % d])
                                    V(lambda e, pc_=pc_: e.scalar_tensor_tensor(out=Hf[:], in0=Hf[:], scalar=pc_, in1=bC[d][:, 128:256], op0=ALU.mult, op1=ALU.add), ["Hf%d" % d, K_("PC"), bCk], ["Hf%d" % d])
                                    if si > 0:
                                        y_accum(d, False, bC[d][:, 0:128], bCk, Yacc, t0 - CTX + c * 64)
                                    yield

                            G(lambda e: e.memset(Yacc[:], 0.0), [], ["Yacc"])
                            for d in range(2):
                                G(lambda e, d=d: e.memset(bk["Hf"][d][:], 0.0), [], ["Hf%d" % d])
                                G(lambda e, d=d: e.memset(bk["Hb"][d][:], 0.0), [], ["Hb%d" % d])
                            run_unit(hg_prep_pre, hg_chain, ORDER_H)
                            if b == 0 and p == 0:
                                dump("Ohg", Yacc[:, :], "Yacc")
                            check("hgchain")
                            for t0 in range(0, SEQ, 512):
                                i = (t0 // 512) % 2
                                sl = slice(t0, t0 + 512)
                                cs = slice(CTX + t0, CTX + t0 + 512)
                                G(lambda e, sl=sl: e.tensor_tensor(out=A_[:, sl], in0=Yacc[:, sl], in1=Yacc[:, sl], op=ALU.mult), ["Yacc"], ["pp_a"])
                                TE(lambda e, i=i, sl=sl: e.matmul(pA[i][:], lhsT=bdones[:], rhs=A_[:, sl], start=True, stop=True), ["bdones", "pp_a"], ["pA%d" % i])
                                V(lambda e, i=i, sl=sl: e.tensor_scalar(out=A_[:, sl], in0=pA[i][:], scalar1=1.0 / 64, scalar2=1e-6, op0=ALU.mult, op1=ALU.add), ["pA%d" % i], ["pp_a"])
                                S(lambda e, sl=sl: e.activation(out=A_[:, sl], in_=A_[:, sl], func=AF.Sqrt), ["pp_a"], ["pp_a"])
                                V(lambda e, sl=sl: e.reciprocal(out=A_[:, sl], in_=A_[:, sl]), ["pp_a"], ["pp_a"])
                                V(lambda e, sl=sl: e.scalar_tensor_tensor(out=Yacc[:, sl], in0=Yacc[:, sl], scalar=pvc("hgn"), in1=A_[:, sl], op0=ALU.mult, op1=ALU.mult), ["Yacc", "pp_a", "pv"], ["Yacc"])
                                V(lambda e, sl=sl, cs=cs: e.tensor_tensor(out=ystage[:, sl], in0=Yacc[:, sl], in1=gg[:, cs], op=ALU.mult), ["Yacc", "gg"], ["ystage"])
                            P.dma(lambda e, p=p: e.dma_start(out=y_d[b, 4 + p], in_=ystage[:]), reads=["ystage"], writes=[("y_d", b * 8 + 4 + p)])
                    P.barrier()
                P.barrier()
                check("mixall")

                with contextlib.ExitStack() as sf:
                    h2T = SB(sf, "h2T", [128, 8, SEQ], BF16)
                    gate = SB(sf, "gate", [128, 16, 65])
                    g2bc = SB(sf, "g2bc", [128, D])
                    P.dma(lambda e: e.dma_start(out=g2bc[:], in_=mod_d[b:b + 1, 5 * D:6 * D].partition_broadcast(128)), writes=["g2bc"])
                    G(lambda e: e.memset(gate[:], 1.0), [], ["gate"])
                    with contextlib.ExitStack() as st:
                        yTs = SB(st, "yTs", [128, 8, SEQ], BF16)
                        wob = SB(st, "wob", [128, 8, D], BF16)
                        g1bc = SB(st, "g1bc", [128, D])
                        xt = [SB(st, "xt%d" % i, [128, D]) for i in range(2)]
                        xn2 = [SB(st, "xn%d" % i, [128, D]) for i in range(2)]
                        junk = SB(st, "junk", [128, D])
                        ssq = [SB(st, "ssq%d" % i, [128, 2]) for i in range(2)]
                        h2f2 = [SB(st, "h2f%d" % i, [128, 8, 128]) for i in range(2)]
                        rt2 = [None, None]
                        r82 = [None, None]
                        rwf = SB(st, "rwf", [128, 8, 64])
                        P.dma(lambda e: e.dma_start(out=rwf[:], in_=router_w.rearrange("(p k) n -> p k n", k=8)), writes=["rwf"])
                        sc_all = SB(st, "sc_all", [128, 16, 64])
                        sel_a = SB(st, "sel_a", [128, 16, 64])
                        tmp_a = SB(st, "tmp_a", [128, 16, 64])
                        m1_a = SB(st, "m1_a", [128, 128])
                        m2_a = SB(st, "m2_a", [128, 128])
                        top_a = SB(st, "top_a", [128, 16, 8])
                        den_a = SB(st, "den_a", [128, 16])
                        pD = [PS(st, "pD%d" % i, [128, D]) for i in range(2)]
                        pT = [PS(st, "pT%d" % i, [128, 4, 128]) for i in range(2)]
                        pR2 = [PS(st, "pR%d" % i, [128, 512]) for i in range(2)]
                        for j in range(8):
                            P.dma(lambda e, j=j: e.dma_start(out=yTs[:, j, :], in_=y_d[b, j]), reads=[("y_d", b * 8 + j)], writes=[("yTs", j)])
                        P.dma(lambda e: e.dma_start(out=wob[:], in_=w_out.rearrange("(k p) n -> p k n", p=128)), writes=["wob"], eng="gpsimd")
                        P.dma(lambda e: e.dma_start(out=g1bc[:], in_=mod_d[b:b + 1, 2 * D:3 * D].partition_broadcast(128)), writes=["g1bc"])
                        DBK = {"xn", "h2f", "pR", "sc", "sel", "eq", "s2", "t1", "em", "m1", "m2", "gs", "top", "gm", "pen", "top8", "den"}
                        for ti in range(16):
                            i = ti % 2
                            tsl = slice(ti * 128, (ti + 1) * 128)
                            xn, h2f, rt, r8, pR = xn2[i], h2f2[i], rt2[i], r82[i], pR2[i]
                            kfix = lambda keys, i=i: [((k[0] + "_%d" % i, k[1]) if k[0] in DBK else k) if isinstance(k, tuple) else (k + "_%d" % i if k in DBK else k) for k in keys]
                            V3 = lambda fn, r=(), w=(): P.op("vector", fn, kfix(r), kfix(w))
                            S3 = lambda fn, r=(), w=(): P.op("scalar", fn, kfix(r), kfix(w))
                            G3 = lambda fn, r=(), w=(): P.op("gpsimd", fn, kfix(r), kfix(w))
                            TE3 = lambda fn, r=(), w=(): P.op("tensor", fn, kfix(r), kfix(w))
                            P.dma(lambda e, i=i, tsl=tsl: e.dma_start(out=xt[i][:], in_=x_in[b, tsl, :]), writes=["xt%d" % i])
                            for hh in range(2):
                                for k in range(8):
                                    TE3(lambda e, i=i, k=k, hh=hh, tsl=tsl: e.matmul(pD[i][:, hh * 512:(hh + 1) * 512], lhsT=yTs[:, k, tsl], rhs=wob[:, k, hh * 512:(hh + 1) * 512],
                                                                                 start=(k == 0), stop=(k == 7)), ["yTs", "wob"], [("pD%d" % i, hh)])
                                hs = slice(hh * 512, (hh + 1) * 512)
                                V3(lambda e, i=i, hs=hs: e.tensor_tensor(out=xn[:, hs], in0=pD[i][:, hs], in1=g1bc[:, hs], op=ALU.mult), [("pD%d" % i, hh), "g1bc"], [("xn", hh)])
                                V3(lambda e, i=i, hs=hs: e.tensor_tensor(out=xt[i][:, hs], in0=xt[i][:, hs], in1=xn[:, hs], op=ALU.add), ["xt%d" % i, ("xn", hh)], ["xt%d" % i])
                            P.dma(lambda e, i=i, tsl=tsl: e.dma_start(out=x1_d[b, tsl, :], in_=xt[i][:]), reads=["xt%d" % i], writes=[("x1_d", b * 16 + ti)])
                            G3(lambda e, i=i: e.memset(ssq[i][:], 0.0), [], ["ssq%d" % i])
                            S3(lambda e, i=i: e.activation(out=junk[:], in_=xt[i][:], func=AF.Square, accum_out=ssq[i][:, 0:1]), ["xt%d" % i], ["junk", "ssq%d" % i])
                            rstd_from_ss(ssq[i], "ssq%d" % i, None)
                            V3(lambda e, i=i: e.tensor_scalar(out=xn[:], in0=xt[i][:], scalar1=ssq[i][:, 1:2], scalar2=None, op0=ALU.mult), ["xt%d" % i, "ssq%d" % i], ["xn"])
                            xv = xn[:].rearrange("t (p k) -> t k p", k=8)
                            for hb_ in range(2):
                                pb = pT[hb_]
                                for k in range(4 * hb_, 4 * hb_ + 4):
                                    TE3(lambda e, k=k, pb=pb: e.matmul(pb[:, k % 4, :], lhsT=xv[:, k, :], rhs=ident[:], start=True, stop=True), ["xn", "ident"], ["pT%d" % hb_])
                                for k in range(4 * hb_, 4 * hb_ + 4):
                                    V3(lambda e, k=k, pb=pb: e.tensor_scalar(out=h2f[:, k, :], in0=pb[:, k % 4, :], scalar1=gs2[:, b, k:k + 1], scalar2=modT[:, b, 3, k:k + 1], op0=ALU.mult, op1=ALU.add),
                                       ["pT%d" % hb_, "gs2", "modT"], [("h2f", k)])
                                for k in range(4 * hb_, 4 * hb_ + 4):
                                    S3(lambda e, k=k, tsl=tsl: e.activation(out=h2T[:, k, tsl], in_=h2f[:, k, :], func=AF.Identity), [("h2f", k)], [("h2T", ti)])
                            for k in range(8):
                                TE3(lambda e, k=k: e.matmul(pR[:, 0:64], lhsT=h2f[:, k, :], rhs=rwf[:, k, :], start=(k == 0), stop=(k == 7)), [("h2f", k), "rwf"], ["pR"])
                            S3(lambda e, ti=ti: e.activation(out=sc_all[:, ti, :], in_=pR[:, 0:64], func=AF.Sigmoid), ["pR"], [("sc_all", ti)])
                        f2 = lambda t: t[:].rearrange("p t e -> p (t e)")
                        g8 = lambda t: t[:].rearrange("p t (g e) -> p (t g) e", e=8)
                        bg = lambda t: t[:].rearrange("p (x o) -> p x o", o=1).to_broadcast([128, 128, 8])
                        rbb = rbbc[:].rearrange("p (o e) -> p o e", o=1).to_broadcast([128, 16, 64])
                        V(lambda e: e.tensor_tensor(out=sel_a[:], in0=sc_all[:], in1=rbb, op=ALU.add), ["sc_all", "rbbc"], ["sel_a"])
                        V(lambda e: e.tensor_reduce(out=m1_a[:], in_=g8(sel_a), axis=AX.X, op=ALU.max), ["sel_a"], ["m1_a"])
                        V(lambda e: e.tensor_tensor(out=g8(tmp_a), in0=g8(sel_a), in1=bg(m1_a), op=ALU.is_ge), ["sel_a", "m1_a"], ["tmp_a"])
                        V(lambda e: e.scalar_tensor_tensor(out=f2(tmp_a), in0=f2(tmp_a), scalar=-1e4, in1=f2(sel_a), op0=ALU.mult, op1=ALU.add), ["tmp_a", "sel_a"], ["tmp_a"])
                        V(lambda e: e.tensor_reduce(out=m2_a[:], in_=g8(tmp_a), axis=AX.X, op=ALU.max), ["tmp_a"], ["m2_a"])
                        V(lambda e: e.tensor_tensor(out=m1_a[:], in0=m1_a[:], in1=m2_a[:], op=ALU.add), ["m1_a", "m2_a"], ["m1_a"])
                        for ti in range(16):
                            V(lambda e, ti=ti: e.max(out=top_a[:, ti, :], in_=m1_a[:, ti * 8:(ti + 1) * 8]), ["m1_a"], [("top_a", ti)])
                        thr4 = top_a[:, :, 3:4].to_broadcast([128, 16, 8])
                        V(lambda e: e.tensor_tensor(out=m2_a[:].rearrange("p (t g) -> p t g", g=8), in0=m1_a[:].rearrange("p (t g) -> p t g", g=8), in1=thr4, op=ALU.is_ge), ["m1_a", "top_a"], ["m2_a"])
                        V(lambda e: e.tensor_scalar(out=m1_a[:], in0=m2_a[:], scalar1=-1.0, scalar2=1e4, op0=ALU.add, op1=ALU.mult), ["m2_a"], ["m1_a"])
                        V(lambda e: e.tensor_tensor(out=g8(tmp_a), in0=g8(sel_a), in1=bg(m2_a), op=ALU.mult), ["sel_a", "m2_a"], ["tmp_a"])
                        V(lambda e: e.tensor_tensor(out=g8(tmp_a), in0=g8(tmp_a), in1=bg(m1_a), op=ALU.add), ["tmp_a", "m1_a"], ["tmp_a"])
                        for ti in range(16):
                            V(lambda e, ti=ti: e.max(out=top_a[:, ti, :], in_=tmp_a[:, ti, :]), ["tmp_a"], [("top_a", ti)])
                        thr8 = top_a[:, :, 7:8].to_broadcast([128, 16, 64])
                        V(lambda e: e.tensor_tensor(out=tmp_a[:], in0=tmp_a[:], in1=thr8, op=ALU.is_ge), ["tmp_a", "top_a"], ["tmp_a"])
                        V(lambda e: e.tensor_tensor(out=tmp_a[:], in0=tmp_a[:], in1=sc_all[:], op=ALU.mult), ["tmp_a", "sc_all"], ["tmp_a"])
                        V(lambda e: e.tensor_reduce(out=den_a[:], in_=tmp_a[:], axis=AX.X, op=ALU.add), ["tmp_a"], ["den_a"])
                        V(lambda e: e.reciprocal(out=den_a[:], in_=den_a[:]), ["den_a"], ["den_a"])
                        rdb = den_a[:].rearrange("p (t o) -> p t o", o=1).to_broadcast([128, 16, 64])
                        V(lambda e: e.scalar_tensor_tensor(out=gate[:, :, 0:64], in0=tmp_a[:], scalar=2.5, in1=rdb, op0=ALU.mult, op1=ALU.mult), ["tmp_a", "den_a"], ["gate"])
                    P.barrier()
                    if b == 0:
                        dump("gate", gate[:].rearrange("p a b -> p (a b)"), "gate")
                        dump("h2T", h2T[:, 0, :], "h2T")
                    check("p3")

                    with contextlib.ExitStack() as st:
                        acc = SB(st, "acc", [128, 16, D])
                        wgb = [SB(st, "wgb%d" % i, [128, 8, 256], BF16) for i in range(3)]
                        wub = [SB(st, "wub%d" % i, [128, 8, 256], BF16) for i in range(3)]
                        wdb = [SB(st, "wdb%d" % i, [128, 2, D], BF16) for i in range(3)]
                        sgt = [SB(st, "sgt%d" % i, [128, 512]) for i in range(2)]
                        actT = [SB(st, "actT%d" % i, [128, 2, 512], BF16) for i in range(2)]
                        xt4 = [SB(st, "xt4_%d" % i, [128, D]) for i in range(4)]
                        junk = SB(st, "junk", [128, D])
                        ss_f = SB(st, "ss_f", [128, 16])
                        fgbc = SB(st, "fgbc", [128, D])
                        P.dma(lambda e: e.dma_start(out=fgbc[:], in_=fng.partition_broadcast(128)), writes=["fgbc"])
                        pG = [PS(st, "pG%d" % i, [128, 512]) for i in range(2)]
                        pU = [PS(st, "pU%d" % i, [128, 512]) for i in range(2)]
                        pD = [PS(st, "pD%d" % i, [128, D]) for i in range(2)]
                        NE = 65
                        pend = None
                        dcount = [0]

                        def emit_down(e_, g, wi, ai, first):
                            for tt in range(4):
                                di = dcount[0] % 2
                                dcount[0] += 1
                                tile_i = g * 4 + tt
                                for hh in range(2):
                                    hs = slice(hh * 512, (hh + 1) * 512)
                                    for k2 in range(2):
                                        TE(lambda e, di=di, hs=hs, k2=k2, tt=tt: e.matmul(pD[di][:, hs], lhsT=actT[ai][:, k2, tt * 128:(tt + 1) * 128], rhs=wdb[wi][:, k2, hs],
                                                                                         start=(k2 == 0), stop=(k2 == 1)), ["actT%d" % ai, "wdb%d" % wi], [("pD%d" % di, hh)])
                                    gcol = gate[:, tile_i, e_:e_ + 1]
                                    if first:
                                        V(lambda e, di=di, hs=hs, gcol=gcol, tile_i=tile_i: e.tensor_scalar(out=acc[:, tile_i, hs], in0=pD[di][:, hs], scalar1=gcol, scalar2=None, op0=ALU.mult),
                                          [("pD%d" % di, hh), "gate"], [("acc", tile_i * 2 + hh)])
                                    else:
                                        V(lambda e, di=di, hs=hs, gcol=gcol, tile_i=tile_i: e.scalar_tensor_tensor(out=acc[:, tile_i, hs], in0=pD[di][:, hs], scalar=gcol, in1=acc[:, tile_i, hs], op0=ALU.mult, op1=ALU.add),
                                          [("pD%d" % di, hh), "gate", ("acc", tile_i * 2 + hh)], [("acc", tile_i * 2 + hh)])

                        cnt = 0
                        for e_ in range(NE):
                            wi = e_ % 3
                            if e_ < 64:
                                srcs = (ewg[e_], ewu[e_], ewd[e_])
                            else:
                                srcs = (swg, swu, swd)
                            P.dma(lambda e, wi=wi, s=srcs[0]: e.dma_start(out=wgb[wi][:], in_=s.rearrange("(p k) n -> p k n", k=8)), writes=["wgb%d" % wi], eng="gpsimd")
                            P.dma(lambda e, wi=wi, s=srcs[1]: e.dma_start(out=wub[wi][:], in_=s.rearrange("(p k) n -> p k n", k=8)), writes=["wub%d" % wi], eng="gpsimd")
                            P.dma(lambda e, wi=wi, s=srcs[2]: e.dma_start(out=wdb[wi][:], in_=s.rearrange("(k p) n -> p k n", p=128)), writes=["wdb%d" % wi], eng="gpsimd")
                            for g in range(4):
                                ai = cnt % 2
                                cnt += 1
                                gsl = slice(g * 512, (g + 1) * 512)
                                for fc in range(2):
                                    fs = slice(fc * 128, (fc + 1) * 128)
                                    for k in range(8):
                                        TE(lambda e, fc=fc, fs=fs, k=k, gsl=gsl: e.matmul(pG[fc][:], lhsT=wgb[wi][:, k, fs], rhs=h2T[:, k, gsl], start=(k == 0), stop=(k == 7)), ["wgb%d" % wi, "h2T"], ["pG%d" % fc])
                                    for k in range(8):
                                        TE(lambda e, fc=fc, fs=fs, k=k, gsl=gsl: e.matmul(pU[fc][:], lhsT=wub[wi][:, k, fs], rhs=h2T[:, k, gsl], start=(k == 0), stop=(k == 7)), ["wub%d" % wi, "h2T"], ["pU%d" % fc])
                                    S(lambda e, fc=fc: e.activation(out=sgt[fc][:], in_=pG[fc][:], func=AF.Silu), ["pG%d" % fc], ["sgt%d" % fc])
                                    V(lambda e, fc=fc, ai=ai: e.tensor_tensor(out=actT[ai][:, fc, :], in0=pU[fc][:], in1=sgt[fc][:], op=ALU.mult), ["pU%d" % fc, "sgt%d" % fc], [("actT%d" % ai, fc)])
                                if pend is not None:
                                    emit_down(*pend)
                                pend = (e_, g, wi, ai, e_ == 0)
                        emit_down(*pend)
                        if b == 0:
                            dump("acc", acc[:, 0, :], "acc")
                        check("moe")
                        G(lambda e: e.memset(ss_f[:], 0.0), [], ["ss_f"])
                        for ti in range(16):
                            i = ti % 4
                            tsl = slice(ti * 128, (ti + 1) * 128)
                            P.dma(lambda e, i=i, tsl=tsl: e.dma_start(out=xt4[i][:], in_=x1_d[b, tsl, :]), reads=[("x1_d", b * 16 + ti)], writes=["xt4_%d" % i])
                            ak = [("acc", ti * 2), ("acc", ti * 2 + 1)]
                            V(lambda e, ti=ti: e.tensor_tensor(out=acc[:, ti, :], in0=acc[:, ti, :], in1=g2bc[:], op=ALU.mult), ak + ["g2bc"], ak)
                            V(lambda e, i=i, ti=ti: e.tensor_tensor(out=acc[:, ti, :], in0=acc[:, ti, :], in1=xt4[i][:], op=ALU.add), ak + ["xt4_%d" % i], ak)
                            S(lambda e, ti=ti: e.activation(out=junk[:], in_=acc[:, ti, :], func=AF.Square, accum_out=ss_f[:, ti:ti + 1]), ak, ["junk", ("ss_f", ti)])
                        V(lambda e: e.tensor_scalar(out=ss_f[:], in0=ss_f[:], scalar1=1.0 / D, scalar2=1e-6, op0=ALU.mult, op1=ALU.add), ["ss_f"], ["ss_f"])
                        S(lambda e: e.activation(out=ss_f[:], in_=ss_f[:], func=AF.Sqrt), ["ss_f"], ["ss_f"])
                        V(lambda e: e.reciprocal(out=ss_f[:], in_=ss_f[:]), ["ss_f"], ["ss_f"])
                        for ti in range(16):
                            tsl = slice(ti * 128, (ti + 1) * 128)
                            ak = [("acc", ti * 2), ("acc", ti * 2 + 1)]
                            V(lambda e, ti=ti: e.scalar_tensor_tensor(out=acc[:, ti, :], in0=acc[:, ti, :], scalar=ss_f[:, ti:ti + 1], in1=fgbc[:], op0=ALU.mult, op1=ALU.mult), ak + ["ss_f", "fgbc"], ak)
                            P.dma(lambda e, ti=ti, tsl=tsl: e.dma_start(out=out_d[b, tsl, :], in_=acc[:, ti, :]), reads=ak, is_output=True)
                        check("b0done")
                    P.barrier()
        stopped = False
        try:
            _body()
        except _Stop:
            stopped = True
        print("emitted", getattr(P, "nemit", 0), {e: len(v) for e, v in P.items.items()})
        if not getattr(P, "replayed", False):
            P.finish()
            with contextlib.ExitStack() as rs:
                P.replay(rs)
        if stopped:
            top.pop_all()
    return nc


def _host_inputs(inp):
    f = lambda a: np.ascontiguousarray(np.asarray(a, dtype=np.float32))
    x = f(inp["x"]); c = f(inp["c"]); ctx = f(inp["ctx"]); c_ctx = f(inp["c_ctx"])
    pvv = np.zeros((128, NPV), np.float32)
    col = lambda v: f(v).reshape(-1, 128).T
    pvv[:, PV["w0"]:PV["w0"] + 8] = col(inp["rw_w0"][0].reshape(-1))
    pvv[:, PV["a0"]:PV["a0"] + 8] = col(inp["rw_a0"][0].reshape(-1))
    pvv[:, PV["k_k"]:PV["k_k"] + 4] = col(inp["rw_k_k"][0])
    pvv[:, PV["k_a"]:PV["k_a"] + 4] = col(inp["rw_k_a"][0])
    pvv[:, PV["r_k"]:PV["r_k"] + 4] = col(inp["rw_r_k"][0].reshape(-1))
    pvv[:, PV["lnx_g"]:PV["lnx_g"] + 4] = col(inp["rw_lnx_g"][0])
    pvv[:, PV["lnx_b"]:PV["lnx_b"] + 4] = col(inp["rw_lnx_b"][0])
    lbl = f(inp["hg_lb_logits"])
    for d in range(2):
        for s in range(2):
            pvv[:, PV["lbl"] + d * 8 + s * 4:PV["lbl"] + d * 8 + s * 4 + 4] = col(lbl[d, s])
    pvv[:, PV["hgn"]] = np.tile(f(inp["hg_norm_g"][0]), 2)
    pvv[:, PV["n1g"]:PV["n1g"] + 8] = f(inp["norm1_g"][0]).reshape(128, 8)
    pvv[:, PV["n2g"]:PV["n2g"] + 8] = f(inp["norm2_g"][0]).reshape(128, 8)
    convw = f(inp["rw_conv"][0]).reshape(9, 12, 128).transpose(2, 1, 0)
    ident = np.eye(128, dtype=np.float32)
    bd = np.zeros((128, 128), np.float32); bd[:64, :64] = 1; bd[64:, 64:] = 1
    r = np.arange(128)[:, None]; cc = np.arange(128)[None, :]
    su = ((r < cc) * bd).astype(np.float32); sl_ = ((r > cc) * bd).astype(np.float32)
    iu = ((r <= cc) * bd).astype(np.float32); il = ((r >= cc) * bd).astype(np.float32)
    z = np.zeros((128, 128), np.float32)
    masks = np.stack([np.concatenate([su, iu, sl_, z], 1), np.concatenate([sl_, il, su, z], 1)], 0)
    shared = dict(
        w_mod=f(inp["w_mod"][0]), b_mod=f(inp["b_mod"][0]).reshape(1, -1), w_in=f(inp["w_in"][0]), convw=np.ascontiguousarray(convw), pv=pvv,
        w2=f(inp["rw_w2"][0]).reshape(128, 512), a2=f(inp["rw_a2"][0]).reshape(128, 512), g2=f(inp["rw_g2"][0]),
        w_out=f(inp["w_out"][0]), router_w=f(inp["router_w"][0]), router_b=f(inp["router_b"][0]).reshape(1, 64),
        ewg=f(inp["exp_w_gate"][0]), ewu=f(inp["exp_w_up"][0]), ewd=f(inp["exp_w_down"][0]),
        swg=f(inp["sh_w_gate"][0]), swu=f(inp["sh_w_up"][0]), swd=f(inp["sh_w_down"][0]),
        fng=f(inp["final_norm_g"]).reshape(1, -1), ident=ident, bdones=bd, masks=np.ascontiguousarray(masks))
    maps = []
    for core in range(8):
        b0 = core * NB
        cv = np.stack([c[b0], c[b0 + 1], c_ctx], -1).reshape(128, 8, 3)
        m = dict(shared)
        m["x_in"] = np.ascontiguousarray(x[b0:b0 + NB]); m["ctx_in"] = np.ascontiguousarray(ctx[b0:b0 + NB]); m["cvec"] = np.ascontiguousarray(cv)
        maps.append(m)
    return maps


def kernel(**inputs):
    maps = _host_inputs(inputs)
    nc = build_program()
    res = run_bass_kernel_spmd(nc, maps, core_ids=list(range(8)))
    return np.concatenate([r["out"] for r in res.results], axis=0).astype(np.float32)
```

```python
import contextlib
import numpy as np
import concourse.bass as bass
import concourse.mybir as mybir
from concourse.bass_utils import run_bass_kernel_spmd

F32 = mybir.dt.float32
BF16 = mybir.dt.bfloat16
AF = mybir.ActivationFunctionType
ALU = mybir.AluOpType
AX = mybir.AxisListType

NB = 2
SEQ = 2048
CTX = 256
TT = SEQ + CTX
D = 1024
LAM = float(np.exp(-0.5))
DEBUG = {}


class _Stop(Exception):
    pass


class _Rec:
    def __init__(self):
        self.call = None

    def __getattr__(self, name):
        def m(*a, **k):
            self.call = (name, a, k)
            return self
        return m


class Prog:
    ENGS = ("tensor", "vector", "scalar", "gpsimd", "sync")
    NDMA = 40

    def __init__(self, nc):
        self.nc = nc
        self.items = {e: [] for e in self.ENGS}
        self.count = {e: 0 for e in self.ENGS}
        self.seen = {e: {} for e in self.ENGS}
        self.state = {}
        self.names = {}
        self.dma_rr = {"sync": 0, "gpsimd": 0}
        self.dma_rng = {"sync": (0, 28), "gpsimd": (28, 40)}
        self.dma_cnt = [0] * self.NDMA
        self.out_events = []

    def _keys(self, k):
        if not isinstance(k, tuple):
            k = (k, None)
        name, sub = k
        subs = self.names.setdefault(name, set())
        if sub is None:
            return [(name, s) for s in subs | {None}]
        subs.add(sub)
        return [(name, sub), (name, None)]

    def _deps(self, reads, writes):
        ev = []
        for k in reads:
            for kk in self._keys(k):
                st = self.state.get(kk)
                if st and st[0] is not None:
                    ev.append(st[0])
        for k in writes:
            for kk in self._keys(k):
                st = self.state.get(kk)
                if st:
                    if st[0] is not None:
                        ev.append(st[0])
                    ev.extend(st[1])
        return ev

    def _commit(self, reads, writes, event):
        for k in reads:
            kk = k if isinstance(k, tuple) else (k, None)
            self._keys(k)
            st = self.state.setdefault(kk, [None, []])
            st[1].append(event)
            if len(st[1]) > 64:
                best = {}
                for (sk, v) in st[1]:
                    if best.get(sk, 0) < v:
                        best[sk] = v
                st[1] = list(best.items())
        for k in writes:
            kk = k if isinstance(k, tuple) else (k, None)
            if kk[1] is None:
                for s in self._keys(k):
                    self.state[s] = [event, []]
            else:
                self._keys(k)
                self.state[kk] = [event, []]

    def _waits(self, eng, events):
        need = {}
        for (sk, val) in events:
            if sk == ("e", "tensor") and eng == "tensor":
                continue
            if self.seen[eng].get(sk, 0) >= val:
                continue
            need[sk] = max(need.get(sk, 0), val)
        for sk, val in need.items():
            self.seen[eng][sk] = val
        return list(need.items())

    def _lim(self):
        import os
        self.nemit = getattr(self, "nemit", 0) + 1
        lim = int(os.environ.get("PLIMIT", "0"))
        if lim and self.nemit > lim:
            self.disabled = True

    _BANK = ("pT", "bA", "bB", "bC", "pA", "pm", "pR", "pG", "pU")

    def _norm(self, ks):
        return [k[0] if isinstance(k, tuple) and k[0][:2] in self._BANK else k for k in ks]

    def op(self, eng, fn, reads=(), writes=()):
        reads, writes = self._norm(reads), self._norm(writes)
        self._lim()
        if getattr(self, "disabled", False):
            return None
        ev = self._deps(reads, writes)
        waits = self._waits(eng, ev)
        self.count[eng] += 1
        event = (("e", eng), self.count[eng])
        rec = _Rec()
        fn(rec)
        self.items[eng].append((waits, rec.call, ("e", eng), 1))
        self._commit(reads, writes, event)
        return event

    def dma(self, fn, reads=(), writes=(), eng="sync", is_output=False):
        self._lim()
        if getattr(self, "disabled", False):
            return None
        ev = self._deps(reads, writes)
        lo, hi = self.dma_rng[eng]
        i = lo + self.dma_rr[eng]
        self.dma_rr[eng] = (self.dma_rr[eng] + 1) % (hi - lo)
        sk = ("d", i)
        if self.dma_cnt[i] > 0:
            ev.append((sk, self.dma_cnt[i]))
        waits = self._waits(eng, ev)
        self.dma_cnt[i] += 16
        event = (sk, self.dma_cnt[i])
        rec = _Rec()
        fn(rec)
        self.items[eng].append((waits, rec.call, sk, 16))
        self._commit(reads, writes, event)
        if is_output:
            self.out_events.append(event)
        return event

    def barrier(self):
        if getattr(self, "disabled", False):
            return
        evs = [(("e", e), self.count[e]) for e in self.ENGS if self.count[e] > 0]
        evs += [(("d", i), self.dma_cnt[i]) for i in range(self.NDMA) if self.dma_cnt[i] > 0]
        for e in self.ENGS:
            w = self._waits(e, evs)
            if w:
                self.items[e].append((w, None, None, 0))
        self.state = {}
        self.names = {}

    def finish(self):
        evs = list(self.out_events)
        evs += [(("d", i), self.dma_cnt[i]) for i in range(self.NDMA) if self.dma_cnt[i] > 0]
        evs += [(("e", e), self.count[e]) for e in self.ENGS if self.count[e] > 0]
        w = self._waits("sync", evs)
        if w:
            self.items["sync"].append((w, None, None, 0))

    def replay(self, stack):
        nc = self.nc
        sems = {}
        for e in self.ENGS:
            sems[("e", e)] = stack.enter_context(nc.semaphore("pe_" + e))
        for i in range(self.NDMA):
            sems[("d", i)] = stack.enter_context(nc.semaphore("pd_%d" % i))
        block = stack.enter_context(nc.Block())
        items = self.items

        def run(engname):
            def body(eng):
                for (waits, fn, sk, inc) in items[engname]:
                    for (wk, val) in waits:
                        eng.wait_ge(sems[wk], val)
                    if fn is not None:
                        ins = getattr(eng, fn[0])(*fn[1], **fn[2])
                        ins.then_inc(sems[sk], inc)
            return body

        block.tensor(run("tensor"))
        block.vector(run("vector"))
        block.scalar(run("scalar"))
        block.gpsimd(run("gpsimd"))
        block.sync(run("sync"))


PV = {}
_o = 0
for _n, _w in (("w0", 8), ("a0", 8), ("k_k", 4), ("k_a", 4), ("omka", 4), ("r_k", 4), ("lnx_g", 4), ("lnx_b", 4),
               ("lbl", 16), ("hgn", 1), ("n1g", 8), ("n2g", 8)):
    PV[_n] = _o
    _o += _w
NPV = _o


def build_program(stop_after=None):
    nc = bass.Bass("TRN2", target_bir_lowering=False)
    dt_in = lambda name, shape: nc.dram_tensor(name, shape, F32, kind="ExternalInput").ap()
    x_in = dt_in("x_in", [NB, SEQ, D])
    ctx_in = dt_in("ctx_in", [NB, CTX, D])
    cvec_in = dt_in("cvec", [128, 8, 3])
    w_mod = dt_in("w_mod", [D, 6 * D])
    b_mod = dt_in("b_mod", [1, 6 * D])
    w_in = dt_in("w_in", [D, 4480])
    convw_in = dt_in("convw", [128, 12, 9])
    pv_in = dt_in("pv", [128, NPV])
    w2_in = dt_in("w2", [128, 512])
    a2_in = dt_in("a2", [128, 512])
    g2_in = dt_in("g2", [128, 512])
    w_out = dt_in("w_out", [D, D])
    router_w = dt_in("router_w", [D, 64])
    router_b = dt_in("router_b", [1, 64])
    ewg = dt_in("ewg", [64, D, 256])
    ewu = dt_in("ewu", [64, D, 256])
    ewd = dt_in("ewd", [64, 256, D])
    swg = dt_in("swg", [D, 256])
    swu = dt_in("swu", [D, 256])
    swd = dt_in("swd", [256, D])
    fng = dt_in("fng", [1, D])
    ident_in = dt_in("ident", [128, 128])
    bdones_in = dt_in("bdones", [128, 128])
    masks_in = dt_in("masks", [2, 128, 512])
    out_d = nc.dram_tensor("out", [NB, SEQ, D], F32, kind="ExternalOutput").ap()
    mod_d = nc.dram_tensor("mod_d", [3, 6 * D], F32).ap()
    y_d = nc.dram_tensor("y_d", [NB, 8, 128, SEQ], BF16).ap()
    x1_d = nc.dram_tensor("x1_d", [NB, SEQ, D], F32).ap()
    dbg = {}
    for name, shape in DEBUG.items():
        dbg[name] = nc.dram_tensor("dbg_" + name, list(shape), F32, kind="ExternalOutput").ap()

    P = Prog(nc)
    V = lambda fn, r=(), w=(): P.op("vector", fn, r, w)
    S = lambda fn, r=(), w=(): P.op("scalar", fn, r, w)
    G = lambda fn, r=(), w=(): P.op("gpsimd", fn, r, w)
    TE = lambda fn, r=(), w=(): P.op("tensor", fn, r, w)

    def dump(name, src_ap, key):
        if name in dbg:
            P.dma(lambda e: e.dma_start(out=dbg[name], in_=src_ap), reads=[key], is_output=True, eng="gpsimd")

    w_in_v = w_in.rearrange("(p k) n -> p k n", k=8)

    with contextlib.ExitStack() as top:
        uid = [0]

        def SB(st, name, shape, dt=F32):
            uid[0] += 1
            return st.enter_context(nc.sbuf_tensor("%s_u%d" % (name, uid[0]), list(shape), dt))

        def PS(st, name, shape, dt=F32):
            uid[0] += 1
            return st.enter_context(nc.psum_tensor("%s_u%d" % (name, uid[0]), list(shape), dt))

        ident = SB(top, "ident", [128, 128])
        identb = SB(top, "identb", [128, 128], BF16)
        bdones = SB(top, "bdones", [128, 128])
        masks = SB(top, "masks", [128, 2, 512])
        pv = SB(top, "pv", [128, NPV])
        convw = SB(top, "convw", [128, 12, 9])
        w2b = SB(top, "w2b", [128, 512], BF16)
        a2b = SB(top, "a2b", [128, 512], BF16)
        g2b = SB(top, "g2b", [128, 512], BF16)
        rbbc = SB(top, "rbbc", [128, 64])
        modT = SB(top, "modT", [128, 3, 6, 8])
        gs1 = SB(top, "gs1", [128, 3, 8])
        gs2 = SB(top, "gs2", [128, 2, 8])
        lbv = SB(top, "lbv", [128, 2, 4, 3])
        P.dma(lambda e: e.dma_start(out=ident[:], in_=ident_in), writes=["ident"])
        P.dma(lambda e: e.dma_start(out=identb[:], in_=ident_in), writes=["identb"], eng="gpsimd")
        P.dma(lambda e: e.dma_start(out=bdones[:], in_=bdones_in), writes=["bdones"])
        for d in range(2):
            P.dma(lambda e, d=d: e.dma_start(out=masks[:, d, :], in_=masks_in[d]), writes=[("masks", d)])
        P.dma(lambda e: e.dma_start(out=pv[:], in_=pv_in), writes=["pv"])
        P.dma(lambda e: e.dma_start(out=convw[:], in_=convw_in), writes=["convw"])
        P.dma(lambda e: e.dma_start(out=w2b[:], in_=w2_in), writes=["w2b"], eng="gpsimd")
        P.dma(lambda e: e.dma_start(out=a2b[:], in_=a2_in), writes=["a2b"], eng="gpsimd")
        P.dma(lambda e: e.dma_start(out=g2b[:], in_=g2_in), writes=["g2b"], eng="gpsimd")
        P.dma(lambda e: e.dma_start(out=rbbc[:], in_=router_b.partition_broadcast(128)), writes=["rbbc"])
        pvc = lambda name, i=0, n=1: pv[:, PV[name] + i:PV[name] + i + n]
        V(lambda e: e.tensor_scalar(out=pvc("omka", 0, 4), in0=pvc("k_a", 0, 4), scalar1=-1.0, scalar2=1.0, op0=ALU.mult, op1=ALU.add), ["pv"], ["pv"])
        for d in range(2):
            lb0 = pv[:, PV["lbl"] + d * 8:PV["lbl"] + d * 8 + 4]
            lb1 = pv[:, PV["lbl"] + d * 8 + 4:PV["lbl"] + d * 8 + 8]
            V(lambda e, d=d, lb0=lb0, lb1=lb1: e.tensor_tensor(out=lbv[:, d, :, 0], in0=lb0, in1=lb1, op=ALU.subtract), ["pv"], ["lbv"])
            S(lambda e, d=d: e.activation(out=lbv[:, d, :, 0], in_=lbv[:, d, :, 0], func=AF.Sigmoid), ["lbv"], ["lbv"])
            V(lambda e, d=d: e.tensor_scalar(out=lbv[:, d, :, 1], in0=lbv[:, d, :, 0], scalar1=-1.0, scalar2=1.0, op0=ALU.mult, op1=ALU.add), ["lbv"], ["lbv"])
            V(lambda e, d=d: e.tensor_scalar(out=lbv[:, d, :, 2], in0=lbv[:, d, :, 1], scalar1=-1.0, scalar2=None, op0=ALU.mult), ["lbv"], ["lbv"])

        with contextlib.ExitStack() as st:
            cv = SB(st, "cv", [128, 8, 3])
            wm = [SB(st, "wm%d" % i, [128, 8, 512]) for i in range(4)]
            bm = [SB(st, "bm%d" % i, [3, 512]) for i in range(2)]
            mo = [SB(st, "mo%d" % i, [3, 512]) for i in range(2)]
            pm = [PS(st, "pm%d" % i, [128, 512]) for i in range(2)]
            P.dma(lambda e: e.dma_start(out=cv[:], in_=cvec_in), writes=["cv"])
            S(lambda e: e.activation(out=cv[:], in_=cv[:], func=AF.Silu), ["cv"], ["cv"])
            wmv = w_mod.rearrange("(p k) n -> p k n", k=8)
            import os
            for g in range(0 if os.environ.get("P0SKIP") else 12):
                i = g % 2
                wi_ = g % 4
                P.dma(lambda e, g=g, wi_=wi_: e.dma_start(out=wm[wi_][:], in_=wmv[:, :, g * 512:(g + 1) * 512]), writes=["wm%d" % wi_], eng=("gpsimd" if g % 2 else "sync"))
                P.dma(lambda e, g=g, i=i: e.dma_start(out=bm[i][:], in_=b_mod[:, g * 512:(g + 1) * 512].partition_broadcast(3)), writes=["bm%d" % i])
                for k in range(8):
                    TE(lambda e, i=i, k=k, wi_=wi_: e.matmul(pm[i][0:3, :], lhsT=cv[:, k, :], rhs=wm[wi_][:, k, :], start=(k == 0), stop=(k == 7)),
                       ["cv", "wm%d" % wi_], ["pm%d" % i])
                V(lambda e, i=i: e.tensor_tensor(out=mo[i][:], in0=pm[i][0:3, :], in1=bm[i][:], op=ALU.add), ["pm%d" % i, "bm%d" % i], ["mo%d" % i])
                P.dma(lambda e, g=g, i=i: e.dma_start(out=mod_d[:, g * 512:(g + 1) * 512], in_=mo[i][:]), reads=["mo%d" % i], writes=["mod_d"])
            for r in range(3):
                P.dma(lambda e, r=r: e.dma_start(out=modT[:, r], in_=mod_d[r].rearrange("(m p k) -> p m k", m=6, k=8)), reads=["mod_d"], writes=[("modT", r)])
            for r in range(3):
                V(lambda e, r=r: e.scalar_tensor_tensor(out=gs1[:, r, :], in0=modT[:, r, 1, :], scalar=1.0, in1=pvc("n1g", 0, 8), op0=ALU.add, op1=ALU.mult),
                  [("modT", r), "pv"], [("gs1", r)])
            for b in range(2):
                V(lambda e, b=b: e.scalar_tensor_tensor(out=gs2[:, b, :], in0=modT[:, b, 4, :], scalar=1.0, in1=pvc("n2g", 0, 8), op0=ALU.add, op1=ALU.mult),
                  [("modT", b), "pv"], [("gs2", b)])
        P.barrier()
        dump("modT", modT[:].rearrange("p a b c -> p (a b c)"), "modT")

        def rstd_from_ss(ss, key, eps_scale):
            V(lambda e: e.tensor_scalar(out=ss[:, 1:2], in0=ss[:, 0:1], scalar1=1.0 / D, scalar2=1e-6, op0=ALU.mult, op1=ALU.add), [key], [key])
            S(lambda e: e.activation(out=ss[:, 1:2], in_=ss[:, 1:2], func=AF.Sqrt), [key], [key])
            V(lambda e: e.reciprocal(out=ss[:, 1:2], in_=ss[:, 1:2]), [key], [key])

        def check(tag):
            if stop_after == tag and not getattr(P, "disabled", False):
                P.finish()
                with contextlib.ExitStack() as rs0:
                    P.replay(rs0)
                P.replayed = True
                P.disabled = True

        def _body():
            for b in range(NB):
                if stop_after == "mod":
                    break
                with contextlib.ExitStack() as sa:
                    hT = SB(sa, "hT", [128, 8, TT], BF16)
                    twl = SB(sa, "twl", [128, TT], BF16)
                    adl = SB(sa, "adl", [128, TT], BF16)
                    gsg = SB(sa, "gsg", [128, TT], BF16)
                    with contextlib.ExitStack() as st:
                        xt = [SB(st, "xt%d" % i, [128, D]) for i in range(2)]
                        junk = SB(st, "junk", [128, D])
                        ssq = [SB(st, "ssq%d" % i, [128, 2]) for i in range(2)]
                        pT = [PS(st, "pT%d" % i, [128, 4, 128]) for i in range(2)]
                        import os
                        for ti in range(int(os.environ.get("P1TILES", "18"))):
                            i = ti % 2
                            if ti < 2:
                                src = ctx_in[b, ti * 128:(ti + 1) * 128, :]
                                r = 2
                            else:
                                src = x_in[b, (ti - 2) * 128:(ti - 1) * 128, :]
                                r = b
                            P.dma(lambda e, i=i, src=src: e.dma_start(out=xt[i][:], in_=src), writes=["xt%d" % i])
                            G(lambda e, i=i: e.memset(ssq[i][:], 0.0), [], ["ssq%d" % i])
                            S(lambda e, i=i: e.activation(out=junk[:], in_=xt[i][:], func=AF.Square, accum_out=ssq[i][:, 0:1]), ["xt%d" % i], ["junk", "ssq%d" % i])
                            rstd_from_ss(ssq[i], "ssq%d" % i, None)
                            V(lambda e, i=i: e.tensor_scalar(out=xt[i][:], in0=xt[i][:], scalar1=ssq[i][:, 1:2], scalar2=None, op0=ALU.mult), ["xt%d" % i, "ssq%d" % i], ["xt%d" % i])
                            xv = xt[i][:].rearrange("t (p k) -> t k p", k=8)
                            for hb_ in range(2):
                                pb = pT[hb_]
                                for k in range(4 * hb_, 4 * hb_ + 4):
                                    TE(lambda e, k=k, pb=pb, xv=xv: e.matmul(pb[:, k % 4, :], lhsT=xv[:, k, :], rhs=ident[:], start=True, stop=True),
                                       ["xt%d" % i, "ident"], ["pT%d" % hb_])
                                for k in range(4 * hb_, 4 * hb_ + 4):
                                    S(lambda e, k=k, pb=pb, r=r, ti=ti: e.activation(out=hT[:, k, ti * 128:(ti + 1) * 128], in_=pb[:, k % 4, :], func=AF.Identity,
                                                                            scale=gs1[:, r, k:k + 1], bias=modT[:, r, 0, k:k + 1]),
                                      ["pT%d" % hb_, "gs1", "modT"], [("hT", ti)])
                    P.barrier()
                    if b == 0:
                        dump("hT", hT[:, 0, :], "hT")
                    check("p1")

                    ipc = [0]

                    def inproj(st_keys, wtile, wkey, wcol, evac, pA):
                        for ti, t0 in enumerate(range(0, TT, 512)):
                            n = min(512, TT - t0)
                            i = ipc[0] % 2
                            ipc[0] += 1
                            for k in range(8):
                                TE(lambda e, i=i, k=k, t0=t0, n=n: e.matmul(pA[i][:, 0:n], lhsT=wtile[:, k, wcol:wcol + 128], rhs=hT[:, k, t0:t0 + n],
                                                                      start=(k == 0), stop=(k == 7)), ["hT", wkey], ["pA%d" % i])
                            evac(pA[i], "pA%d" % i, t0, n)

                    with contextlib.ExitStack() as st:
                        wl = SB(st, "wl", [128, 8, 384], BF16)
                        pA = [PS(st, "pA%d" % i, [128, 512]) for i in range(2)]
                        P.dma(lambda e: e.dma_start(out=wl[:], in_=w_in_v[:, :, 1536:1920]), writes=["wl"], eng="gpsimd")
                        inproj(None, wl, "wl", 0, lambda ps, pk, t0, n: S(lambda e: e.activation(out=twl[:, t0:t0 + n], in_=ps[:, 0:n], func=AF.Tanh), [pk], ["twl"]), pA)
                        inproj(None, wl, "wl", 128, lambda ps, pk, t0, n: S(lambda e: e.activation(out=adl[:, t0:t0 + n], in_=ps[:, 0:n], func=AF.Identity), [pk], ["adl"]), pA)
                        inproj(None, wl, "wl", 256, lambda ps, pk, t0, n: S(lambda e: e.activation(out=gsg[:, t0:t0 + n], in_=ps[:, 0:n], func=AF.Sigmoid), [pk], ["gsg"]), pA)
                    P.barrier()

                    check("lora")
                    SEGS = [(0, 4)] + [(256 + 256 * i_, 4) for i_ in range(8)]
                    ORDER = {0: list(range(9)), 1: [0] + list(range(8, 0, -1))}

                    def decay_prep(tp, d, lw, lwkey, n, want_exc, sc=1.0):
                        nch = n // 64
                        gz = tp["gz"]
                        kx = lambda nm: nm + tp["sfx"]
                        V(lambda e: e.tensor_tensor_scan(out=gz[:, 1:1 + n], data0=tp["ones"][:, 0:n], data1=lw, initial=0.0, op0=ALU.mult, op1=ALU.add),
                          [lwkey, "ones"], [kx("gz")])
                        gv = lambda off: gz[:, off:off + n].rearrange("p (c j) -> p c j", j=64)
                        v3 = lambda t: t[:, 0:n].rearrange("p (c j) -> p c j", j=64)
                        if d == 0:
                            base = gv(0)[:, :, 0:1].to_broadcast([128, nch, 64])
                            V(lambda e: e.tensor_tensor(out=v3(tp["Dinc"]), in0=gv(1), in1=base, op=ALU.subtract), [kx("gz")], [kx("Dinc")])
                            if want_exc:
                                V(lambda e: e.tensor_tensor(out=v3(tp["Dexc"]), in0=gv(0), in1=base, op=ALU.subtract), [kx("gz")], [kx("Dexc")])
                        else:
                            top_ = gz[:, 64:64 + n].rearrange("p (c j) -> p c j", j=64)[:, :, 0:1].to_broadcast([128, nch, 64])
                            V(lambda e: e.tensor_tensor(out=v3(tp["Dinc"]), in0=top_, in1=gv(0), op=ALU.subtract), [kx("gz")], [kx("Dinc")])
                            if want_exc:
                                V(lambda e: e.tensor_tensor(out=v3(tp["Dexc"]), in0=top_, in1=gv(1), op=ALU.subtract), [kx("gz")], [kx("Dexc")])
                        S(lambda e: e.activation(out=tp["Eii"][:, 0:n], in_=tp["Dinc"][:, 0:n], func=AF.Exp, scale=-sc), [kx("Dinc")], [kx("Eii")])
                        S(lambda e: e.activation(out=tp["Dinc"][:, 0:n], in_=tp["Dinc"][:, 0:n], func=AF.Exp, scale=sc), [kx("Dinc")], [kx("Dinc")])
                        if want_exc:
                            S(lambda e: e.activation(out=tp["Dexc"][:, 0:n], in_=tp["Dexc"][:, 0:n], func=AF.Exp, scale=sc), [kx("Dexc")], [kx("Dexc")])

                    def rr_run(gens):
                        gens = list(gens)
                        while gens:
                            for g_ in list(gens):
                                try:
                                    next(g_)
                                except StopIteration:
                                    gens.remove(g_)

                    def run_unit(prep_pre, chain, order=None):
                        order = ORDER if order is None else order
                        ns = len(order[0])
                        rr_run([prep_pre(order[d][0], d, 0) for d in range(2)])
                        for i_ in range(ns):
                            gens = [chain(order[d][i_], d, i_ % 2) for d in range(2)]
                            if i_ + 1 < ns:
                                gens += [prep_pre(order[d][i_ + 1], d, (i_ + 1) % 2) for d in range(2)]
                            rr_run(gens)

                    SEGS_H = [(0, 4)] + [(256 + 512 * i_, 8) for i_ in range(4)]
                    ORDER_H = {0: list(range(5)), 1: [0] + list(range(4, 0, -1))}

                    def bd_write(eng, dst, dkey, col0, src, skeys, E, ekey, n, neg=False):
                        nch = n // 64
                        for u in range(2):
                            o = dst[u * 64:(u + 1) * 64, 0:nch, col0 + u * 64:col0 + (u + 1) * 64]
                            a = src[u * 64:(u + 1) * 64, :].rearrange("p (c j) -> p c j", j=64)
                            bb = E[u * 64:(u + 1) * 64, 0:n].rearrange("p (c j) -> p c j", j=64)
                            if neg:
                                P.op(eng, lambda e, o=o, a=a, bb=bb: e.scalar_tensor_tensor(out=o, in0=a, scalar=-1.0, in1=bb, op0=ALU.mult, op1=ALU.mult),
                                     list(skeys) + [ekey], [dkey])
                            else:
                                P.op(eng, lambda e, o=o, a=a, bb=bb: e.tensor_tensor(out=o, in0=a, in1=bb, op=ALU.mult), list(skeys) + [ekey], [dkey])

                    def h_update(d, bk, pHt, pHk, Hf, Hb, pc_ap, pckey):
                        HP = bk["HP"][d]
                        G(lambda e: e.tensor_scalar(out=HP[:], in0=Hf[:], scalar1=pc_ap, scalar2=None, op0=ALU.mult), ["Hf%d" % d, pckey], ["HP%d" % d])
                        V(lambda e: e.scalar_tensor_tensor(out=Hb[:], in0=pHt, scalar=pc_ap, in1=HP[:], op0=ALU.mult, op1=ALU.add), [pHk, pckey, "HP%d" % d], ["Hb%d" % d])
                        V(lambda e: e.scalar_tensor_tensor(out=Hf[:], in0=pHt, scalar=pc_ap, in1=HP[:], op0=ALU.mult, op1=ALU.add), [pHk, pckey, "HP%d" % d], ["Hf%d" % d])

                    def y_accum(d, first, pYt, pYk, Yacc, tok0):
                        dst = Yacc[:, tok0:tok0 + 64]
                        if True:
                            V(lambda e: e.tensor_tensor(out=dst, in0=dst, in1=pYt[:, 0:64], op=ALU.add), [pYk, ("Yacc", tok0)], [("Yacc", tok0)])
                            V(lambda e: e.tensor_tensor(out=dst, in0=dst, in1=pYt[:, 64:128], op=ALU.add), [pYk, ("Yacc", tok0)], [("Yacc", tok0)])

                    with contextlib.ExitStack() as su:
                        wu = SB(su, "wu", [128, 8, 384], BF16)
                        c0 = SB(su, "c0", [128, 2504], BF16)
                        dg = SB(su, "dg", [128, 9, 128], BF16)
                        rc = SB(su, "rc", [128, TT], BF16)
                        kc = SB(su, "kc", [128, TT], BF16)
                        vc = SB(su, "vc", [128, TT], BF16)
                        kk = SB(su, "kk", [128, TT], BF16)
                        kds = SB(su, "kds", [128, SEQ], BF16)
                        Yacc = SB(su, "Yacc", [128, SEQ])
                        tps = [{n_: SB(su, "tp%d_%s" % (d, n_), [128, 256]) for n_ in ("lw", "asg", "t1", "kd", "bb", "Dinc", "Dexc", "Eii")} for d in range(2)]
                        ones = SB(su, "tp_ones", [128, 256])
                        G(lambda e: e.memset(ones[:], 1.0), [], ["ones"])
                        for d in range(2):
                            tps[d]["gz"] = SB(su, "tp%d_gz" % d, [128, 384])
                            tps[d]["ones"] = ones
                            tps[d]["sfx"] = str(d)
                            G(lambda e, d=d: e.memset(tps[d]["gz"][:, 0:1], 0.0), [], ["gz%d" % d])
                        bk = {}
                        for nm, w_ in (("AR", 256), ("BT", 128), ("KT", 128), ("VT", 128), ("Bt", 128), ("Kt", 128), ("Vt", 128), ("nt2", 256), ("kt2", 256), ("mt", 128)):
                            bk[nm] = [[SB(su, "%s%d_%d" % (nm, d, s_), [128, 4, w_], BF16) for s_ in range(3 if nm in ("AR", "BT", "KT", "VT") else 2)] for d in range(2)]
                        bk["PC"] = [[SB(su, "PC%d_%d" % (d, s_), [128, 4]) for s_ in range(3)] for d in range(2)]
                        bk["Hf"] = [SB(su, "Hf%d" % d, [128, 128]) for d in range(2)]
                        bk["Hb"] = [SB(su, "Hb%d" % d, [128, 128], BF16) for d in range(2)]
                        bk["HP"] = [SB(su, "HP%d" % d, [128, 128]) for d in range(2)]
                        wkd = {nm: [SB(su, "%s_%d" % (nm, d), [128, 4, 128], BF16) for d in range(2)] for nm in ("nm", "xs0", "xs1", "xts0", "xts1")}
                        rhsb = [[SB(su, "rhs%d_%d" % (d, s_), [128, 128], BF16) for s_ in range(2)] for d in range(2)]
                        ubb = [[SB(su, "ub%d_%d" % (d, s_), [128, 128], BF16) for s_ in range(2)] for d in range(2)]
                        for d in range(2):
                            for s_ in range(3):
                                for nm in ("AR", "BT", "KT", "VT"):
                                    G(lambda e, nm=nm, d=d, s_=s_: e.memset(bk[nm][d][s_][:], 0.0), [], ["%s%d_p%d" % (nm, d, s_)])
                        print("RWKV scope sbuf remaining", nc.sbuf_bytes_remaining)
                        pA = [PS(su, "pA%d" % i, [128, 512]) for i in range(2)]
                        bC = [PS(su, "bC%d" % d, [128, 512]) for d in range(2)]
                        Wb = [PS(su, "bW%d" % i, [128, 512]) for i in range(4)]
                        pp = {"a": SB(su, "pp_a", [128, 512]), "b": SB(su, "pp_b", [128, 512])}

                        for p in range(4):
                            if stop_after == "h":
                                break
                            for j, col in enumerate((p * 128, 512 + p * 128, 1024 + p * 128)):
                                P.dma(lambda e, j=j, col=col: e.dma_start(out=wu[:, :, j * 128:(j + 1) * 128], in_=w_in_v[:, :, col:col + 128]), writes=["wu"], eng="gpsimd")
                            LAT0 = 259
                            G(lambda e: e.memset(c0[:], 0.0), [], ["c0"])
                            latv = c0[:, LAT0:LAT0 + 34 * 66].rearrange("p (y x) -> p y x", x=66)
                            idb9 = identb[:].rearrange("p (o x) -> p o x", o=1).to_broadcast([128, 9, 128])
                            for j, dstc in enumerate((rc, kc, vc)):
                                dk = ("rc", "kc", "vc")[j]
                                cwb = convw[:, j * 4 + p, :].rearrange("p (t o) -> p t o", o=1).to_broadcast([128, 9, 128])
                                V(lambda e, cwb=cwb: e.tensor_tensor(out=dg[:], in0=idb9, in1=cwb, op=ALU.mult), ["identb", "convw"], ["dg"])

                                def evac_pad(ps, pk, t0, n):
                                    if t0 == 0:
                                        S(lambda e: e.activation(out=c0[:, 1:1 + CTX], in_=ps[:, 0:CTX], func=AF.Identity), [pk], ["c0"])
                                        lo, r0 = CTX, 0
                                    else:
                                        lo, r0 = 0, (t0 - CTX) // 64
                                    nr = (n - lo) // 64
                                    S(lambda e: e.activation(out=latv[:, 1 + r0:1 + r0 + nr, 1:65], in_=ps[:, lo:n].rearrange("p (y x) -> p y x", x=64), func=AF.Identity), [pk], ["c0"])
                                inproj(None, wu, "wu", j * 128, evac_pad, pA)
                                wi_ = 0
                                wt, wtk = Wb[wi_ % 4], "bW%d" % (wi_ % 4)
                                for bb_ in range(3):
                                    TE(lambda e, bb_=bb_, wt=wt: e.matmul(wt[:, 0:CTX], lhsT=dg[:, 3 + bb_, :], rhs=c0[:, bb_:bb_ + CTX], start=(bb_ == 0), stop=(bb_ == 2)), ["dg", "c0"], [wtk])
                                S(lambda e, wt=wt, dstc=dstc: e.activation(out=dstc[:, 0:CTX], in_=wt[:, 0:CTX], func=AF.Identity), [wtk], [dk])
                                r0 = 0
                                while r0 < 32:
                                    nr = min(7, 32 - r0)
                                    wi_ += 1
                                    wt, wtk = Wb[wi_ % 4], "bW%d" % (wi_ % 4)
                                    nn = nr * 66
                                    ti_ = 0
                                    for a in range(3):
                                        for bb_ in range(3):
                                            off = LAT0 + (1 + r0 + a - 1) * 66 + (bb_ - 1)
                                            TE(lambda e, a=a, bb_=bb_, off=off, nn=nn, wt=wt, ti_=ti_: e.matmul(wt[:, 0:nn], lhsT=dg[:, a * 3 + bb_, :], rhs=c0[:, off:off + nn], start=(ti_ == 0), stop=(ti_ == 8)),
                                               ["dg", "c0"], [wtk])
                                            ti_ += 1
                                    src = wt[:, 0:nn].rearrange("p (y x) -> p y x", x=66)[:, :, 1:65]
                                    dst = dstc[:, CTX + r0 * 64:CTX + (r0 + nr) * 64].rearrange("p (y x) -> p y x", x=64)
                                    S(lambda e, src=src, dst=dst: e.activation(out=dst, in_=src, func=AF.Identity), [wtk], [dk])
                                    r0 += nr
                            if b == 0 and p == 0:
                                dump("rc", rc[:, :], "rc")
                                dump("vc", vc[:, :], "vc")
                            for t0 in range(0, TT, 512):
                                n = min(512, TT - t0)
                                i = (t0 // 512) % 2
                                V(lambda e, t0=t0, n=n: e.tensor_scalar(out=c0[:, t0:t0 + n], in0=kc[:, t0:t0 + n], scalar1=pvc("k_k", p), scalar2=None, op0=ALU.mult), ["kc", "pv"], [("c0", t0)])
                                G(lambda e, t0=t0, n=n: e.tensor_tensor(out=pp["a"][:, 0:n], in0=c0[:, t0:t0 + n], in1=c0[:, t0:t0 + n], op=ALU.mult), [("c0", t0)], ["pp_a"])
                                TE(lambda e, i=i, n=n: e.matmul(pA[i][:, 0:n], lhsT=bdones[:], rhs=pp["a"][:, 0:n], start=True, stop=True), ["bdones", "pp_a"], ["pA%d" % i])
                                S(lambda e, i=i, n=n: e.activation(out=pp["b"][:, 0:n], in_=pA[i][:, 0:n], func=AF.Sqrt), ["pA%d" % i], ["pp_b"])
                                V(lambda e, n=n: e.tensor_scalar(out=pp["b"][:, 0:n], in0=pp["b"][:, 0:n], scalar1=1e-12, scalar2=None, op0=ALU.max), ["pp_b"], ["pp_b"])
                                V(lambda e, n=n: e.reciprocal(out=pp["b"][:, 0:n], in_=pp["b"][:, 0:n]), ["pp_b"], ["pp_b"])
                                V(lambda e, t0=t0, n=n: e.tensor_tensor(out=kk[:, t0:t0 + n], in0=c0[:, t0:t0 + n], in1=pp["b"][:, 0:n], op=ALU.mult), [("c0", t0), "pp_b"], [("kk", t0)])
                            if b == 0 and p == 0:
                                dump("kk", kk[:, :], "kk")
                            check("kk")

                            def rw_prep(si, d, slot, pslot):
                                tp = tps[d]
                                sf = "%d_%d" % (d, slot)
                                K_ = lambda nm: (nm + "%d_p%d" % (d, pslot)) if nm in ("AR", "BT", "KT", "VT", "PC") else (nm + sf)
                                tk = lambda nm: nm + str(d)
                                t0, nch = SEGS[si]
                                n = nch * 64
                                sl = slice(t0, t0 + n)
                                dd = slice(d * 64, (d + 1) * 64)
                                fcol = slice(p * 128, (p + 1) * 128)
                                pa, pak = pA[d], "pA%d" % d
                                AR, BT, KT, VT = (bk[nm][d][pslot] for nm in ("AR", "BT", "KT", "VT"))
                                Bt, Kt, Vt = (bk[nm][d][slot] for nm in ("Bt", "Kt", "Vt"))
                                nt2, kt2, mt = (bk[nm][d][slot] for nm in ("nt2", "kt2", "mt"))
                                Wa, Wc = Wb[2 * d], Wb[2 * d + 1]
                                Wak, Wck = "bW%d" % (2 * d), "bW%d" % (2 * d + 1)
                                TE(lambda e: e.matmul(pa[:, 0:n], lhsT=w2b[dd, fcol], rhs=twl[dd, sl], start=True, stop=True), ["w2b", "twl"], [pak])
                                TE(lambda e: e.matmul(pa[:, 256:256 + n], lhsT=a2b[dd, fcol], rhs=adl[dd, sl], start=True, stop=True), ["a2b", "adl"], [pak])
                                S(lambda e: e.activation(out=tp["lw"][:, 0:n], in_=pa[:, 0:n], func=AF.Sigmoid, bias=pvc("w0", d * 4 + p)), [pak, "pv"], [tk("lw")])
                                S(lambda e: e.activation(out=tp["asg"][:, 0:n], in_=pa[:, 256:256 + n], func=AF.Sigmoid, bias=pvc("a0", d * 4 + p)), [pak, "pv"], [tk("asg")])
                                yield
                                decay_prep(tp, d, tp["lw"][:, 0:n], tk("lw"), n, True, sc=-LAM)
                                yield
                                S(lambda e: e.activation(out=tp["t1"][:, 0:n], in_=tp["asg"][:, 0:n], func=AF.Identity, scale=pvc("k_a", p), bias=pvc("omka", p)), [tk("asg"), "pv"], [tk("t1")])
                                G(lambda e: e.tensor_tensor(out=tp["kd"][:, 0:n], in0=tp["t1"][:, 0:n], in1=kc[:, sl], op=ALU.mult), [tk("t1"), "kc"], [tk("kd")])
                                V(lambda e: e.tensor_tensor(out=tp["bb"][:, 0:n], in0=tp["asg"][:, 0:n], in1=kk[:, sl], op=ALU.mult), [tk("asg"), "kk"], [tk("bb")])
                                if si > 0:
                                    ks = kds[:, t0 - CTX:t0 - CTX + n]
                                    G(lambda e: e.tensor_tensor(out=ks, in0=ks, in1=tp["kd"][:, 0:n], op=ALU.add), [tk("kd"), ("kds", si)], [("kds", si)])
                                yield
                                bd_write("vector", AR, K_("AR"), 0, kk[:, sl], ["kk"], tp["Dexc"], tk("Dexc"), n, neg=True)
                                bd_write("gpsimd", AR, K_("AR"), 128, rc[:, sl], ["rc"], tp["Dinc"], tk("Dinc"), n)
                                bd_write("vector", BT, K_("BT"), 0, tp["bb"][:, 0:n], [tk("bb")], tp["Eii"], tk("Eii"), n)
                                bd_write("vector", KT, K_("KT"), 0, tp["kd"][:, 0:n], [tk("kd")], tp["Eii"], tk("Eii"), n)
                                pcsrc = tp["Dinc"][:, 0:n].rearrange("p (c j) -> p c j", j=64)[:, :, (63 if d == 0 else 0)]
                                V(lambda e: e.tensor_copy(out=bk["PC"][d][pslot][:, 0:nch], in_=pcsrc), [tk("Dinc")], [K_("PC")])
                                for u in range(2):
                                    o = VT[u * 64:(u + 1) * 64, 0:nch, u * 64:(u + 1) * 64]
                                    a_ = vc[u * 64:(u + 1) * 64, sl].rearrange("p (c j) -> p c j", j=64)
                                    S(lambda e, o=o, a_=a_: e.activation(out=o, in_=a_, func=AF.Identity), ["vc"], [K_("VT")])
                                yield

                            def rw_pre(si, d, slot, pslot):
                                tp = tps[d]
                                sf = "%d_%d" % (d, slot)
                                K_ = lambda nm: (nm + "%d_p%d" % (d, pslot)) if nm in ("AR", "BT", "KT", "VT", "PC") else (nm + sf)
                                tk = lambda nm: nm + str(d)
                                t0, nch = SEGS[si]
                                n = nch * 64
                                sl = slice(t0, t0 + n)
                                dd = slice(d * 64, (d + 1) * 64)
                                fcol = slice(p * 128, (p + 1) * 128)
                                pa, pak = pA[d], "pA%d" % d
                                AR, BT, KT, VT = (bk[nm][d][pslot] for nm in ("AR", "BT", "KT", "VT"))
                                Bt, Kt, Vt = (bk[nm][d][slot] for nm in ("Bt", "Kt", "Vt"))
                                nt2, kt2, mt = (bk[nm][d][slot] for nm in ("nt2", "kt2", "mt"))
                                Wa, Wc = Wb[2 * d], Wb[2 * d + 1]
                                Wak, Wck = "bW%d" % (2 * d), "bW%d" % (2 * d + 1)
                                m2b = masks[:, d:d + 1, 0:256].to_broadcast([128, 2, 256])
                                m1b = masks[:, d:d + 1, 256:384].to_broadcast([128, 4, 128])
                                idb4 = identb[:].rearrange("p (o x) -> p o x", o=1).to_broadcast([128, 4, 128])
                                w2v = lambda wt: wt[:].rearrange("p (c x) -> p c x", x=256)
                                w4v = lambda wt: wt[:].rearrange("p (c x) -> p c x", x=128)
                                for lhs_, lk, dst, dk in ((BT, K_("BT"), nt2, K_("nt2")), (KT, K_("KT"), kt2, K_("kt2"))):
                                    for j in range(4):
                                        wt, wtk = (Wa, Wak) if j < 2 else (Wc, Wck)
                                        TE(lambda e, j=j, wt=wt, lhs_=lhs_: e.matmul(wt[:, (j % 2) * 256:(j % 2 + 1) * 256], lhsT=lhs_[:, j, :], rhs=AR[:, j, :], start=True, stop=True), [lk, K_("AR")], [wtk])
                                    V(lambda e, dst=dst: e.tensor_tensor(out=dst[:, 0:2, :], in0=w2v(Wa), in1=m2b, op=ALU.mult), [Wak, "masks"], [dk])
                                    V(lambda e, dst=dst: e.tensor_tensor(out=dst[:, 2:4, :], in0=w2v(Wc), in1=m2b, op=ALU.mult), [Wck, "masks"], [dk])
                                    yield
                                nm_ = wkd["nm"][d]
                                for j in range(4):
                                    TE(lambda e, j=j: e.matmul(Wa[:, j * 128:(j + 1) * 128], lhsT=AR[:, j, 0:128], rhs=BT[:, j, :], start=True, stop=True), [K_("AR"), K_("BT")], [Wak])
                                for j in range(4):
                                    TE(lambda e, j=j: e.matmul(Wc[:, j * 128:(j + 1) * 128], lhsT=BT[:, j, :], rhs=identb[:], start=True, stop=True), [K_("BT"), "identb"], [Wck])
                                V(lambda e: e.tensor_tensor(out=nm_[:], in0=w4v(Wa), in1=m1b, op=ALU.mult), [Wak, "masks"], [tk("nm")])
                                S(lambda e: e.activation(out=Bt[:], in_=w4v(Wc), func=AF.Identity), [Wck], [K_("Bt")])
                                yield
                                for j in range(4):
                                    TE(lambda e, j=j: e.matmul(Wa[:, j * 128:(j + 1) * 128], lhsT=KT[:, j, :], rhs=identb[:], start=True, stop=True), [K_("KT"), "identb"], [Wak])
                                for j in range(4):
                                    TE(lambda e, j=j: e.matmul(Wc[:, j * 128:(j + 1) * 128], lhsT=VT[:, j, :], rhs=identb[:], start=True, stop=True), [K_("VT"), "identb"], [Wck])
                                S(lambda e: e.activation(out=Kt[:], in_=w4v(Wa), func=AF.Identity), [Wak], [K_("Kt")])
                                V(lambda e: e.tensor_copy(out=Vt[:], in_=w4v(Wc)), [Wck], [K_("Vt")])
                                V(lambda e: e.tensor_tensor(out=mt[:], in0=nt2[:, :, 0:128], in1=idb4, op=ALU.add), [K_("nt2"), "identb"], [K_("mt")])
                                yield
                                X, XT, Xk, XTk = nm_, nt2[:, :, 0:128], tk("nm"), K_("nt2")
                                import os
                                for j in range(1, 1 if os.environ.get("RWSKIP") == "inv" else 6):
                                    xn_, xtn_ = wkd["xs%d" % (j % 2)][d], wkd["xts%d" % (j % 2)][d]
                                    xnk, xtnk = tk("xs%d" % (j % 2)), tk("xts%d" % (j % 2))
                                    for c in range(4):
                                        TE(lambda e, c=c, X=X, XT=XT: e.matmul(Wa[:, c * 128:(c + 1) * 128], lhsT=XT[:, c, :], rhs=X[:, c, :], start=True, stop=True), [Xk, XTk], [Wak])
                                    if j < 5:
                                        for c in range(4):
                                            TE(lambda e, c=c, X=X, XT=XT: e.matmul(Wc[:, c * 128:(c + 1) * 128], lhsT=X[:, c, :], rhs=XT[:, c, :], start=True, stop=True), [Xk, XTk], [Wck])
                                    S(lambda e, xn_=xn_: e.activation(out=xn_[:], in_=w4v(Wa), func=AF.Identity), [Wak], [xnk])
                                    if j < 5:
                                        S(lambda e, xtn_=xtn_: e.activation(out=xtn_[:], in_=w4v(Wc), func=AF.Identity), [Wck], [xtnk])
                                    yield
                                    for c in range(4):
                                        TE(lambda e, c=c: e.matmul(Wa[:, c * 128:(c + 1) * 128], lhsT=identb[:], rhs=mt[:, c, :], start=True, stop=False), ["identb", K_("mt")], [Wak])
                                        TE(lambda e, c=c, xn_=xn_: e.matmul(Wa[:, c * 128:(c + 1) * 128], lhsT=xn_[:, c, :], rhs=mt[:, c, :], start=False, stop=True), [xnk, K_("mt")], [Wak])
                                    S(lambda e: e.activation(out=mt[:], in_=w4v(Wa), func=AF.Identity), [Wak, K_("mt")], [K_("mt")])
                                    X, XT, Xk, XTk = xn_[:], xtn_[:], xnk, xtnk
                                    yield

                            def rw_chain(si, d, slot, pslot):
                                sf = "%d_%d" % (d, slot)
                                K_ = lambda nm: (nm + "%d_p%d" % (d, pslot)) if nm in ("AR", "BT", "KT", "VT", "PC") else (nm + sf)
                                t0, nch = SEGS[si]
                                AR = bk["AR"][d][pslot]
                                Bt, Kt, Vt = (bk[nm][d][slot] for nm in ("Bt", "Kt", "Vt"))
                                nt2, kt2, mt = (bk[nm][d][slot] for nm in ("nt2", "kt2", "mt"))
                                Hf, Hb = bk["Hf"][d], bk["Hb"][d]
                                bCk = "bC%d" % d
                                order = list(range(nch)) if d == 0 else list(range(nch))[::-1]
                                import os
                                if os.environ.get("RWSKIP") == "chain":
                                    return
                                for ci, c in enumerate(order):
                                    rb, ub = rhsb[d][ci % 2], ubb[d][ci % 2]
                                    rbk, ubk = "rhs%d_%d" % (d, ci % 2), "ub%d_%d" % (d, ci % 2)
                                    TE(lambda e, c=c: e.matmul(bC[d][:, 0:128], lhsT=AR[:, c, 0:128], rhs=Hb[:], start=True, stop=False), [K_("AR"), "Hb%d" % d], [bCk])
                                    TE(lambda e, c=c: e.matmul(bC[d][:, 0:128], lhsT=kt2[:, c, 0:128], rhs=Vt[:, c, :], start=False, stop=True), [K_("kt2"), K_("Vt")], [bCk])
                                    S(lambda e, rb=rb: e.activation(out=rb[:], in_=bC[d][:, 0:128], func=AF.Identity), [bCk], [rbk])
                                    yield
                                    TE(lambda e, c=c, rb=rb: e.matmul(bC[d][:, 128:256], lhsT=mt[:, c, :], rhs=rb[:], start=True, stop=True), [K_("mt"), rbk], [bCk])
                                    S(lambda e, ub=ub: e.activation(out=ub[:], in_=bC[d][:, 128:256], func=AF.Identity), [bCk], [ubk])
                                    yield
                                    if si > 0:
                                        TE(lambda e, c=c: e.matmul(bC[d][:, 256:384], lhsT=Hb[:], rhs=AR[:, c, 128:256], start=True, stop=False), [K_("AR"), "Hb%d" % d], [bCk])
                                        TE(lambda e, c=c, ub=ub: e.matmul(bC[d][:, 256:384], lhsT=ub[:], rhs=nt2[:, c, 128:256], start=False, stop=False), [ubk, K_("nt2")], [bCk])
                                        TE(lambda e, c=c: e.matmul(bC[d][:, 256:384], lhsT=Vt[:, c, :], rhs=kt2[:, c, 128:256], start=False, stop=True), [K_("Vt"), K_("kt2")], [bCk])
                                    TE(lambda e, c=c, ub=ub: e.matmul(bC[d][:, 384:512], lhsT=Bt[:, c, :], rhs=ub[:], start=True, stop=False), [K_("Bt"), ubk], [bCk])
                                    TE(lambda e, c=c: e.matmul(bC[d][:, 384:512], lhsT=Kt[:, c, :], rhs=Vt[:, c, :], start=False, stop=True), [K_("Kt"), K_("Vt")], [bCk])
                                    h_update(d, bk, bC[d][:, 384:512], bCk, Hf, Hb, bk["PC"][d][pslot][:, c:c + 1], K_("PC"))
                                    if si > 0:
                                        y_accum(d, False, bC[d][:, 256:384], bCk, Yacc, t0 - CTX + c * 64)
                                    yield

                            G(lambda e: e.memset(kds[:], 0.0), [], ["kds"])
                            G(lambda e: e.memset(Yacc[:], 0.0), [], ["Yacc"])
                            for d in range(2):
                                G(lambda e, d=d: e.memset(bk["Hf"][d][:], 0.0), [], ["Hf%d" % d])
                                G(lambda e, d=d: e.memset(bk["Hb"][d][:], 0.0), [], ["Hb%d" % d])
                            ns_ = len(ORDER[0])
                            rr_run([rw_prep(ORDER[d][0], d, 0, 0) for d in range(2)])
                            rr_run([rw_pre(ORDER[d][0], d, 0, 0) for d in range(2)] + [rw_prep(ORDER[d][1], d, 0, 1) for d in range(2)])
                            for i_ in range(ns_):
                                gens = [rw_chain(ORDER[d][i_], d, i_ % 2, i_ % 3) for d in range(2)]
                                if i_ + 1 < ns_:
                                    gens += [rw_pre(ORDER[d][i_ + 1], d, (i_ + 1) % 2, (i_ + 1) % 3) for d in range(2)]
                                if i_ + 2 < ns_:
                                    gens += [rw_prep(ORDER[d][i_ + 2], d, 0, (i_ + 2) % 3) for d in range(2)]
                                rr_run(gens)
                            if b == 0 and p == 0:
                                dump("Yacc", Yacc[:, :], "Yacc")
                            check("chain")
                            A_, B_ = pp["a"], pp["b"]
                            for t0 in range(0, SEQ, 512):
                                i = (t0 // 512) % 2
                                sl = slice(t0, t0 + 512)
                                cs = slice(CTX + t0, CTX + t0 + 512)
                                wbn, wbnk = Wb[i], "bW%d" % i
                                wgt, wgtk = Wb[2 + i], "bW%d" % (2 + i)
                                V(lambda e, sl=sl, cs=cs: e.scalar_tensor_tensor(out=B_[:, 0:512], in0=rc[:, cs], scalar=pvc("r_k", p), in1=kds[:, sl], op0=ALU.mult, op1=ALU.mult), ["rc", "kds", "pv"], ["pp_b"])
                                TE(lambda e, wbn=wbn: e.matmul(wbn[:], lhsT=bdones[:], rhs=B_[:, 0:512], start=True, stop=True), ["bdones", "pp_b"], [wbnk])
                                TE(lambda e, wgt=wgt, cs=cs: e.matmul(wgt[:], lhsT=g2b[:, p * 128:(p + 1) * 128], rhs=gsg[:, cs], start=True, stop=True), ["g2b", "gsg"], [wgtk])
                                TE(lambda e, i=i, sl=sl: e.matmul(pA[i][:], lhsT=bdones[:], rhs=Yacc[:, sl], start=True, stop=True), ["bdones", "Yacc"], ["pA%d" % i])
                                V(lambda e, i=i, sl=sl: e.scalar_tensor_tensor(out=Yacc[:, sl], in0=pA[i][:], scalar=-1.0 / 64, in1=Yacc[:, sl], op0=ALU.mult, op1=ALU.add), ["pA%d" % i, "Yacc"], ["Yacc"])
                                V(lambda e, sl=sl: e.tensor_tensor(out=A_[:, 0:512], in0=Yacc[:, sl], in1=Yacc[:, sl], op=ALU.mult), ["Yacc"], ["pp_a"])
                                TE(lambda e, i=i, sl=sl: e.matmul(pA[i][:], lhsT=bdones[:], rhs=A_[:, 0:512], start=True, stop=True), ["bdones", "pp_a"], ["pA%d" % i])
                                V(lambda e, i=i, sl=sl: e.tensor_scalar(out=A_[:, 0:512], in0=pA[i][:], scalar1=1.0 / 64, scalar2=64e-5, op0=ALU.mult, op1=ALU.add), ["pA%d" % i], ["pp_a"])
                                S(lambda e, sl=sl: e.activation(out=A_[:, 0:512], in_=A_[:, 0:512], func=AF.Sqrt), ["pp_a"], ["pp_a"])
                                V(lambda e, sl=sl: e.reciprocal(out=A_[:, 0:512], in_=A_[:, 0:512]), ["pp_a"], ["pp_a"])
                                V(lambda e, sl=sl: e.tensor_tensor(out=Yacc[:, sl], in0=Yacc[:, sl], in1=A_[:, 0:512], op=ALU.mult), ["Yacc", "pp_a"], ["Yacc"])
                                V(lambda e, sl=sl: e.tensor_scalar(out=Yacc[:, sl], in0=Yacc[:, sl], scalar1=pvc("lnx_g", p), scalar2=pvc("lnx_b", p), op0=ALU.mult, op1=ALU.add), ["Yacc", "pv"], ["Yacc"])
                                V(lambda e, wbn=wbn, cs=cs: e.tensor_tensor(out=B_[:, 0:512], in0=wbn[:], in1=vc[:, cs], op=ALU.mult), [wbnk, "vc"], ["pp_b"])
                                V(lambda e, sl=sl: e.tensor_tensor(out=Yacc[:, sl], in0=Yacc[:, sl], in1=B_[:, 0:512], op=ALU.add), ["Yacc", "pp_b"], ["Yacc"])
                                V(lambda e, wgt=wgt, sl=sl: e.tensor_tensor(out=c0[:, sl], in0=wgt[:], in1=Yacc[:, sl], op=ALU.mult), [wgtk, "Yacc"], ["c0"])
                            P.dma(lambda e, p=p: e.dma_start(out=y_d[b, p], in_=c0[:, 0:SEQ]), reads=["c0"], writes=[("y_d", b * 8 + p)])
                    P.barrier()
                    check("rwall")

                    with contextlib.ExitStack() as su:
                        wu = SB(su, "wu", [128, 8, 640], BF16)
                        qs = SB(su, "qs", [128, TT], BF16)
                        vs = SB(su, "vs", [128, TT], BF16)
                        gg = SB(su, "gg", [128, TT], BF16)
                        fsg = [SB(su, "fsg%d" % d, [128, TT]) for d in range(2)]
                        Yacc = SB(su, "Yacc", [128, SEQ])
                        tps = [{n_: SB(su, "tp%d_%s" % (d, n_), [128, 512]) for n_ in ("lw", "kf", "Dinc", "Eii")} for d in range(2)]
                        ones = SB(su, "tp_ones", [128, 512])
                        G(lambda e: e.memset(ones[:], 1.0), [], ["ones"])
                        for d in range(2):
                            tps[d]["gz"] = SB(su, "tp%d_gz" % d, [128, 640])
                            tps[d]["ones"] = ones
                            tps[d]["sfx"] = str(d)
                            G(lambda e, d=d: e.memset(tps[d]["gz"][:, 0:1], 0.0), [], ["gz%d" % d])
                        bk = {}
                        for nm in ("QT", "KT", "VT", "Kt", "Vt", "st"):
                            bk[nm] = [[SB(su, "%s%d_%d" % (nm, d, s_), [128, 8, 128], BF16) for s_ in range(2)] for d in range(2)]
                        bk["PC"] = [[SB(su, "PC%d_%d" % (d, s_), [128, 8]) for s_ in range(2)] for d in range(2)]
                        KHs = [SB(su, "KH%d" % d, [128, 8, 128], BF16) for d in range(2)]
                        bk["Hf"] = [SB(su, "Hf%d" % d, [128, 128]) for d in range(2)]
                        bk["Hb"] = [SB(su, "Hb%d" % d, [128, 128], BF16) for d in range(2)]
                        bk["HP"] = [SB(su, "HP%d" % d, [128, 128]) for d in range(2)]
                        for d in range(2):
                            for s_ in range(2):
                                for nm in ("QT", "KT", "VT"):
                                    G(lambda e, nm=nm, d=d, s_=s_: e.memset(bk[nm][d][s_][:], 0.0), [], ["%s%d_%d" % (nm, d, s_)])
                        pA = [PS(su, "pA%d" % i, [128, 512]) for i in range(2)]
                        bC = [PS(su, "bC%d" % d, [128, 512]) for d in range(2)]
                        Wb = [PS(su, "bW%d" % i, [128, 512]) for i in range(4)]
                        ystage = SB(su, "ystage", [128, SEQ], BF16)
                        A_ = SB(su, "pp_a", [128, SEQ])
                        for p in range(4):
                            for j, col in enumerate((1920, 2432, 2944, 3456, 3968)):
                                P.dma(lambda e, j=j, col=col: e.dma_start(out=wu[:, :, j * 128:(j + 1) * 128], in_=w_in_v[:, :, col + p * 128:col + (p + 1) * 128]), writes=["wu"], eng="gpsimd")
                            inproj(None, wu, "wu", 0, lambda ps, pk, t0, n: S(lambda e: e.activation(out=A_[:, 0:n], in_=ps[:, 0:n], func=AF.Silu), [pk], ["pp_a"]) and
                                   V(lambda e: e.tensor_scalar(out=qs[:, t0:t0 + n], in0=A_[:, 0:n], scalar1=0.125, scalar2=None, op0=ALU.mult), ["pp_a"], ["qs"]), pA)
                            inproj(None, wu, "wu", 128, lambda ps, pk, t0, n: S(lambda e: e.activation(out=fsg[0][:, t0:t0 + n], in_=ps[:, 0:n], func=AF.Sigmoid), [pk], ["fsg0"]), pA)
                            inproj(None, wu, "wu", 256, lambda ps, pk, t0, n: S(lambda e: e.activation(out=fsg[1][:, t0:t0 + n], in_=ps[:, 0:n], func=AF.Sigmoid), [pk], ["fsg1"]), pA)
                            inproj(None, wu, "wu", 384, lambda ps, pk, t0, n: S(lambda e: e.activation(out=vs[:, t0:t0 + n], in_=ps[:, 0:n], func=AF.Identity), [pk], ["vs"]), pA)
                            inproj(None, wu, "wu", 512, lambda ps, pk, t0, n: S(lambda e: e.activation(out=gg[:, t0:t0 + n], in_=ps[:, 0:n], func=AF.Silu), [pk], ["gg"]), pA)

                            check("hgin")
                            def hg_prep_pre(si, d, slot):
                                tp = tps[d]
                                sf = "%d_%d" % (d, slot)
                                K_ = lambda nm: nm + sf
                                tk = lambda nm: nm + str(d)
                                t0, nch = SEGS_H[si]
                                n = nch * 64
                                sl = slice(t0, t0 + n)
                                QT, KT, VT, Kt, Vt, st_ = (bk[nm][d][slot] for nm in ("QT", "KT", "VT", "Kt", "Vt", "st"))
                                Wa, Wc = Wb[2 * d], Wb[2 * d + 1]
                                Wak, Wck = "bW%d" % (2 * d), "bW%d" % (2 * d + 1)
                                V(lambda e: e.tensor_scalar(out=tp["lw"][:, 0:n], in0=fsg[d][:, sl], scalar1=lbv[:, d, p, 1:2], scalar2=lbv[:, d, p, 0:1], op0=ALU.mult, op1=ALU.add), ["fsg%d" % d, "lbv"], [tk("lw")])
                                S(lambda e: e.activation(out=tp["lw"][:, 0:n], in_=tp["lw"][:, 0:n], func=AF.Ln), [tk("lw")], [tk("lw")])
                                S(lambda e: e.activation(out=tp["kf"][:, 0:n], in_=fsg[d][:, sl], func=AF.Identity, scale=lbv[:, d, p, 2:3], bias=lbv[:, d, p, 1:2]), ["fsg%d" % d, "lbv"], [tk("kf")])
                                yield
                                decay_prep(tp, d, tp["lw"][:, 0:n], tk("lw"), n, False)
                                yield
                                bd_write("vector", QT, K_("QT"), 0, qs[:, sl], ["qs"], tp["Dinc"], tk("Dinc"), n)
                                bd_write("gpsimd", KT, K_("KT"), 0, tp["kf"][:, 0:n], [tk("kf")], tp["Eii"], tk("Eii"), n)
                                pcsrc = tp["Dinc"][:, 0:n].rearrange("p (c j) -> p c j", j=64)[:, :, (63 if d == 0 else 0)]
                                V(lambda e: e.tensor_copy(out=bk["PC"][d][slot][:, 0:nch], in_=pcsrc), [tk("Dinc")], [K_("PC")])
                                KH = KHs[d]
                                pcb = bk["PC"][d][slot][:, 0:nch].rearrange("p (c o) -> p c o", o=1).to_broadcast([128, nch, 128])
                                V(lambda e: e.tensor_tensor(out=KH[:, 0:nch, :], in0=KT[:, 0:nch, :], in1=pcb, op=ALU.mult), [K_("KT"), K_("PC")], [tk("KH")])
                                for u in range(2):
                                    o = VT[u * 64:(u + 1) * 64, 0:nch, u * 64:(u + 1) * 64]
                                    a_ = vs[u * 64:(u + 1) * 64, sl].rearrange("p (c j) -> p c j", j=64)
                                    S(lambda e, o=o, a_=a_: e.activation(out=o, in_=a_, func=AF.Identity), ["vs"], [K_("VT")])
                                yield
                                mIb = masks[:, d:d + 1, 128:256].to_broadcast([128, 4, 128])
                                w4v = lambda wt: wt[:].rearrange("p (c x) -> p c x", x=128)
                                halves = [(0, Wa, Wak)] + ([(4, Wc, Wck)] if nch > 4 else [])
                                for c0_, wt, wtk in halves:
                                    for j in range(4):
                                        TE(lambda e, j=j, c0_=c0_, wt=wt: e.matmul(wt[:, j * 128:(j + 1) * 128], lhsT=KT[:, c0_ + j, :], rhs=QT[:, c0_ + j, :], start=True, stop=True), [K_("KT"), K_("QT")], [wtk])
                                for c0_, wt, wtk in halves:
                                    V(lambda e, c0_=c0_, wt=wt: e.tensor_tensor(out=st_[:, c0_:c0_ + 4, :], in0=w4v(wt), in1=mIb, op=ALU.mult), [wtk, "masks"], [K_("st")])
                                yield
                                for c0_, wt, wtk in halves:
                                    for j in range(4):
                                        TE(lambda e, j=j, c0_=c0_, wt=wt: e.matmul(wt[:, j * 128:(j + 1) * 128], lhsT=KH[:, c0_ + j, :], rhs=identb[:], start=True, stop=True), [tk("KH"), "identb"], [wtk])
                                for c0_, wt, wtk in halves:
                                    S(lambda e, c0_=c0_, wt=wt: e.activation(out=Kt[:, c0_:c0_ + 4, :], in_=w4v(wt), func=AF.Identity), [wtk], [K_("Kt")])
                                yield
                                for c0_, wt, wtk in halves:
                                    for j in range(4):
                                        TE(lambda e, j=j, c0_=c0_, wt=wt: e.matmul(wt[:, j * 128:(j + 1) * 128], lhsT=VT[:, c0_ + j, :], rhs=identb[:], start=True, stop=True), [K_("VT"), "identb"], [wtk])
                                for i_h, (c0_, wt, wtk) in enumerate(halves):
                                    if i_h == 0:
                                        S(lambda e, c0_=c0_, wt=wt: e.activation(out=Vt[:, c0_:c0_ + 4, :], in_=w4v(wt), func=AF.Identity), [wtk], [K_("Vt")])
                                    else:
                                        V(lambda e, c0_=c0_, wt=wt: e.tensor_copy(out=Vt[:, c0_:c0_ + 4, :], in_=w4v(wt)), [wtk], [K_("Vt")])
                                yield

                            def hg_chain(si, d, slot):
                                sf = "%d_%d" % (d, slot)
                                K_ = lambda nm: nm + sf
                                t0, nch = SEGS_H[si]
                                QT, Kt, Vt, st_ = (bk[nm][d][slot] for nm in ("QT", "Kt", "Vt", "st"))
                                Hf, Hb = bk["Hf"][d], bk["Hb"][d]
                                bCk = "bC%d" % d
                                order = list(range(nch)) if d == 0 else list(range(nch))[::-1]
                                for c in order:
                                    if si > 0:
                                        TE(lambda e, c=c: e.matmul(bC[d][:, 0:128], lhsT=Vt[:, c, :], rhs=st_[:, c, :], start=True, stop=False), [K_("Vt"), K_("st")], [bCk])
                                        TE(lambda e, c=c: e.matmul(bC[d][:, 0:128], lhsT=Hb[:], rhs=QT[:, c, :], start=False, stop=True), ["Hb%d" % d, K_("QT")], [bCk])
                                    TE(lambda e, c=c: e.matmul(bC[d][:, 128:256], lhsT=Kt[:, c, :], rhs=Vt[:, c, :], start=True, stop=True), [K_("Kt"), K_("Vt")], [bCk])
                                    pc_ = bk["PC"][d][slot][:, c:c + 1]
                                    V(lambda e, pc_=pc_: e.scalar_tensor_tensor(out=Hb[:], in0=Hf[:], scalar=pc_, in1=bC[d][:, 128:256], op0=ALU.mult, op1=ALU.add), ["Hf%d" % d, K_("PC"), bCk], ["Hb%d" % d])
                                    V(lambda e, pc_=pc_: e.scalar_tensor_tensor(out=Hf[:], in0=Hf[:], scalar=pc_, in1=bC[d][:, 128:256], op0=ALU.mult, op1=ALU.add), ["Hf%d" % d, K_("PC"), bCk], ["Hf%d" % d])
                                    if si > 0:
                                        y_accum(d, False, bC[d][:, 0:128], bCk, Yacc, t0 - CTX + c * 64)
                                    yield

                            G(lambda e: e.memset(Yacc[:], 0.0), [], ["Yacc"])
                            for d in range(2):
                                G(lambda e, d=d: e.memset(bk["Hf"][d][:], 0.0), [], ["Hf%d" % d])
                                G(lambda e, d=d: e.memset(bk["Hb"][d][:], 0.0), [], ["Hb%d" % d])
                            run_unit(hg_prep_pre, hg_chain, ORDER_H)
                            if b == 0 and p == 0:
                                dump("Ohg", Yacc[:, :], "Yacc")
                            check("hgchain")
                            for t0 in range(0, SEQ, 512):
                                i = (t0 // 512) % 2
                                sl = slice(t0, t0 + 512)
                                cs = slice(CTX + t0, CTX + t0 + 512)
                                G(lambda e, sl=sl: e.tensor_tensor(out=A_[:, sl], in0=Yacc[:, sl], in1=Yacc[:, sl], op=ALU.mult), ["Yacc"], ["pp_a"])
                                TE(lambda e, i=i, sl=sl: e.matmul(pA[i][:], lhsT=bdones[:], rhs=A_[:, sl], start=True, stop=True), ["bdones", "pp_a"], ["pA%d" % i])
                                V(lambda e, i=i, sl=sl: e.tensor_scalar(out=A_[:, sl], in0=pA[i][:], scalar1=1.0 / 64, scalar2=1e-6, op0=ALU.mult, op1=ALU.add), ["pA%d" % i], ["pp_a"])
                                S(lambda e, sl=sl: e.activation(out=A_[:, sl], in_=A_[:, sl], func=AF.Sqrt), ["pp_a"], ["pp_a"])
                                V(lambda e, sl=sl: e.reciprocal(out=A_[:, sl], in_=A_[:, sl]), ["pp_a"], ["pp_a"])
                                V(lambda e, sl=sl: e.scalar_tensor_tensor(out=Yacc[:, sl], in0=Yacc[:, sl], scalar=pvc("hgn"), in1=A_[:, sl], op0=ALU.mult, op1=ALU.mult), ["Yacc", "pp_a", "pv"], ["Yacc"])
                                V(lambda e, sl=sl, cs=cs: e.tensor_tensor(out=ystage[:, sl], in0=Yacc[:, sl], in1=gg[:, cs], op=ALU.mult), ["Yacc", "gg"], ["ystage"])
                            P.dma(lambda e, p=p: e.dma_start(out=y_d[b, 4 + p], in_=ystage[:]), reads=["ystage"], writes=[("y_d", b * 8 + 4 + p)])
                    P.barrier()
                P.barrier()
                check("mixall")

                with contextlib.ExitStack() as sf:
                    h2T = SB(sf, "h2T", [128, 8, SEQ], BF16)
                    gate = SB(sf, "gate", [128, 16, 65])
                    g2bc = SB(sf, "g2bc", [128, D])
                    P.dma(lambda e: e.dma_start(out=g2bc[:], in_=mod_d[b:b + 1, 5 * D:6 * D].partition_broadcast(128)), writes=["g2bc"])
                    G(lambda e: e.memset(gate[:], 1.0), [], ["gate"])
                    with contextlib.ExitStack() as st:
                        yTs = SB(st, "yTs", [128, 8, SEQ], BF16)
                        wob = SB(st, "wob", [128, 8, D], BF16)
                        g1bc = SB(st, "g1bc", [128, D])
                        xt = [SB(st, "xt%d" % i, [128, D]) for i in range(2)]
                        xn2 = [SB(st, "xn%d" % i, [128, D]) for i in range(2)]
                        junk = SB(st, "junk", [128, D])
                        ssq = [SB(st, "ssq%d" % i, [128, 2]) for i in range(2)]
                        h2f2 = [SB(st, "h2f%d" % i, [128, 8, 128]) for i in range(2)]
                        rt2 = [None, None]
                        r82 = [None, None]
                        rwf = SB(st, "rwf", [128, 8, 64])
                        P.dma(lambda e: e.dma_start(out=rwf[:], in_=router_w.rearrange("(p k) n -> p k n", k=8)), writes=["rwf"])
                        sc_all = SB(st, "sc_all", [128, 16, 64])
                        sel_a = SB(st, "sel_a", [128, 16, 64])
                        tmp_a = SB(st, "tmp_a", [128, 16, 64])
                        m1_a = SB(st, "m1_a", [128, 128])
                        m2_a = SB(st, "m2_a", [128, 128])
                        top_a = SB(st, "top_a", [128, 16, 8])
                        den_a = SB(st, "den_a", [128, 16])
                        pD = [PS(st, "pD%d" % i, [128, D]) for i in range(2)]
                        pT = [PS(st, "pT%d" % i, [128, 4, 128]) for i in range(2)]
                        pR2 = [PS(st, "pR%d" % i, [128, 512]) for i in range(2)]
                        for j in range(8):
                            P.dma(lambda e, j=j: e.dma_start(out=yTs[:, j, :], in_=y_d[b, j]), reads=[("y_d", b * 8 + j)], writes=[("yTs", j)])
                        P.dma(lambda e: e.dma_start(out=wob[:], in_=w_out.rearrange("(k p) n -> p k n", p=128)), writes=["wob"], eng="gpsimd")
                        P.dma(lambda e: e.dma_start(out=g1bc[:], in_=mod_d[b:b + 1, 2 * D:3 * D].partition_broadcast(128)), writes=["g1bc"])
                        DBK = {"xn", "h2f", "pR", "sc", "sel", "eq", "s2", "t1", "em", "m1", "m2", "gs", "top", "gm", "pen", "top8", "den"}
                        for ti in range(16):
                            i = ti % 2
                            tsl = slice(ti * 128, (ti + 1) * 128)
                            xn, h2f, rt, r8, pR = xn2[i], h2f2[i], rt2[i], r82[i], pR2[i]
                            kfix = lambda keys, i=i: [((k[0] + "_%d" % i, k[1]) if k[0] in DBK else k) if isinstance(k, tuple) else (k + "_%d" % i if k in DBK else k) for k in keys]
                            V3 = lambda fn, r=(), w=(): P.op("vector", fn, kfix(r), kfix(w))
                            S3 = lambda fn, r=(), w=(): P.op("scalar", fn, kfix(r), kfix(w))
                            G3 = lambda fn, r=(), w=(): P.op("gpsimd", fn, kfix(r), kfix(w))
                            TE3 = lambda fn, r=(), w=(): P.op("tensor", fn, kfix(r), kfix(w))
                            P.dma(lambda e, i=i, tsl=tsl: e.dma_start(out=xt[i][:], in_=x_in[b, tsl, :]), writes=["xt%d" % i])
                            for hh in range(2):
                                for k in range(8):
                                    TE3(lambda e, i=i, k=k, hh=hh, tsl=tsl: e.matmul(pD[i][:, hh * 512:(hh + 1) * 512], lhsT=yTs[:, k, tsl], rhs=wob[:, k, hh * 512:(hh + 1) * 512],
                                                                                 start=(k == 0), stop=(k == 7)), ["yTs", "wob"], [("pD%d" % i, hh)])
                                hs = slice(hh * 512, (hh + 1) * 512)
                                V3(lambda e, i=i, hs=hs: e.tensor_tensor(out=xn[:, hs], in0=pD[i][:, hs], in1=g1bc[:, hs], op=ALU.mult), [("pD%d" % i, hh), "g1bc"], [("xn", hh)])
                                V3(lambda e, i=i, hs=hs: e.tensor_tensor(out=xt[i][:, hs], in0=xt[i][:, hs], in1=xn[:, hs], op=ALU.add), ["xt%d" % i, ("xn", hh)], ["xt%d" % i])
                            P.dma(lambda e, i=i, tsl=tsl: e.dma_start(out=x1_d[b, tsl, :], in_=xt[i][:]), reads=["xt%d" % i], writes=[("x1_d", b * 16 + ti)])
                            G3(lambda e, i=i: e.memset(ssq[i][:], 0.0), [], ["ssq%d" % i])
                            S3(lambda e, i=i: e.activation(out=junk[:], in_=xt[i][:], func=AF.Square, accum_out=ssq[i][:, 0:1]), ["xt%d" % i], ["junk", "ssq%d" % i])
                            rstd_from_ss(ssq[i], "ssq%d" % i, None)
                            V3(lambda e, i=i: e.tensor_scalar(out=xn[:], in0=xt[i][:], scalar1=ssq[i][:, 1:2], scalar2=None, op0=ALU.mult), ["xt%d" % i, "ssq%d" % i], ["xn"])
                            xv = xn[:].rearrange("t (p k) -> t k p", k=8)
                            for hb_ in range(2):
                                pb = pT[hb_]
                                for k in range(4 * hb_, 4 * hb_ + 4):
                                    TE3(lambda e, k=k, pb=pb: e.matmul(pb[:, k % 4, :], lhsT=xv[:, k, :], rhs=ident[:], start=True, stop=True), ["xn", "ident"], ["pT%d" % hb_])
                                for k in range(4 * hb_, 4 * hb_ + 4):
                                    V3(lambda e, k=k, pb=pb: e.tensor_scalar(out=h2f[:, k, :], in0=pb[:, k % 4, :], scalar1=gs2[:, b, k:k + 1], scalar2=modT[:, b, 3, k:k + 1], op0=ALU.mult, op1=ALU.add),
                                       ["pT%d" % hb_, "gs2", "modT"], [("h2f", k)])
                                for k in range(4 * hb_, 4 * hb_ + 4):
                                    S3(lambda e, k=k, tsl=tsl: e.activation(out=h2T[:, k, tsl], in_=h2f[:, k, :], func=AF.Identity), [("h2f", k)], [("h2T", ti)])
                            for k in range(8):
                                TE3(lambda e, k=k: e.matmul(pR[:, 0:64], lhsT=h2f[:, k, :], rhs=rwf[:, k, :], start=(k == 0), stop=(k == 7)), [("h2f", k), "rwf"], ["pR"])
                            S3(lambda e, ti=ti: e.activation(out=sc_all[:, ti, :], in_=pR[:, 0:64], func=AF.Sigmoid), ["pR"], [("sc_all", ti)])
                        f2 = lambda t: t[:].rearrange("p t e -> p (t e)")
                        g8 = lambda t: t[:].rearrange("p t (g e) -> p (t g) e", e=8)
                        bg = lambda t: t[:].rearrange("p (x o) -> p x o", o=1).to_broadcast([128, 128, 8])
                        rbb = rbbc[:].rearrange("p (o e) -> p o e", o=1).to_broadcast([128, 16, 64])
                        V(lambda e: e.tensor_tensor(out=sel_a[:], in0=sc_all[:], in1=rbb, op=ALU.add), ["sc_all", "rbbc"], ["sel_a"])
                        V(lambda e: e.tensor_reduce(out=m1_a[:], in_=g8(sel_a), axis=AX.X, op=ALU.max), ["sel_a"], ["m1_a"])
                        V(lambda e: e.tensor_tensor(out=g8(tmp_a), in0=g8(sel_a), in1=bg(m1_a), op=ALU.is_ge), ["sel_a", "m1_a"], ["tmp_a"])
                        V(lambda e: e.scalar_tensor_tensor(out=f2(tmp_a), in0=f2(tmp_a), scalar=-1e4, in1=f2(sel_a), op0=ALU.mult, op1=ALU.add), ["tmp_a", "sel_a"], ["tmp_a"])
                        V(lambda e: e.tensor_reduce(out=m2_a[:], in_=g8(tmp_a), axis=AX.X, op=ALU.max), ["tmp_a"], ["m2_a"])
                        V(lambda e: e.tensor_tensor(out=m1_a[:], in0=m1_a[:], in1=m2_a[:], op=ALU.add), ["m1_a", "m2_a"], ["m1_a"])
                        for ti in range(16):
                            V(lambda e, ti=ti: e.max(out=top_a[:, ti, :], in_=m1_a[:, ti * 8:(ti + 1) * 8]), ["m1_a"], [("top_a", ti)])
                        thr4 = top_a[:, :, 3:4].to_broadcast([128, 16, 8])
                        V(lambda e: e.tensor_tensor(out=m2_a[:].rearrange("p (t g) -> p t g", g=8), in0=m1_a[:].rearrange("p (t g) -> p t g", g=8), in1=thr4, op=ALU.is_ge), ["m1_a", "top_a"], ["m2_a"])
                        V(lambda e: e.tensor_scalar(out=m1_a[:], in0=m2_a[:], scalar1=-1.0, scalar2=1e4, op0=ALU.add, op1=ALU.mult), ["m2_a"], ["m1_a"])
                        V(lambda e: e.tensor_tensor(out=g8(tmp_a), in0=g8(sel_a), in1=bg(m2_a), op=ALU.mult), ["sel_a", "m2_a"], ["tmp_a"])
                        V(lambda e: e.tensor_tensor(out=g8(tmp_a), in0=g8(tmp_a), in1=bg(m1_a), op=ALU.add), ["tmp_a", "m1_a"], ["tmp_a"])
                        for ti in range(16):
                            V(lambda e, ti=ti: e.max(out=top_a[:, ti, :], in_=tmp_a[:, ti, :]), ["tmp_a"], [("top_a", ti)])
                        thr8 = top_a[:, :, 7:8].to_broadcast([128, 16, 64])
                        V(lambda e: e.tensor_tensor(out=tmp_a[:], in0=tmp_a[:], in1=thr8, op=ALU.is_ge), ["tmp_a", "top_a"], ["tmp_a"])
                        V(lambda e: e.tensor_tensor(out=tmp_a[:], in0=tmp_a[:], in1=sc_all[:], op=ALU.mult), ["tmp_a", "sc_all"], ["tmp_a"])
                        V(lambda e: e.tensor_reduce(out=den_a[:], in_=tmp_a[:], axis=AX.X, op=ALU.add), ["tmp_a"], ["den_a"])
                        V(lambda e: e.reciprocal(out=den_a[:], in_=den_a[:]), ["den_a"], ["den_a"])
                        rdb = den_a[:].rearrange("p (t o) -> p t o", o=1).to_broadcast([128, 16, 64])
                        V(lambda e: e.scalar_tensor_tensor(out=gate[:, :, 0:64], in0=tmp_a[:], scalar=2.5, in1=rdb, op0=ALU.mult, op1=ALU.mult), ["tmp_a", "den_a"], ["gate"])
                    P.barrier()
                    if b == 0:
                        dump("gate", gate[:].rearrange("p a b -> p (a b)"), "gate")
                        dump("h2T", h2T[:, 0, :], "h2T")
                    check("p3")

                    with contextlib.ExitStack() as st:
                        acc = SB(st, "acc", [128, 16, D])
                        wgb = [SB(st, "wgb%d" % i, [128, 8, 256], BF16) for i in range(3)]
                        wub = [SB(st, "wub%d" % i, [128, 8, 256], BF16) for i in range(3)]
                        wdb = [SB(st, "wdb%d" % i, [128, 2, D], BF16) for i in range(3)]
                        sgt = [SB(st, "sgt%d" % i, [128, 512]) for i in range(2)]
                        actT = [SB(st, "actT%d" % i, [128, 2, 512], BF16) for i in range(2)]
                        xt4 = [SB(st, "xt4_%d" % i, [128, D]) for i in range(4)]
                        junk = SB(st, "junk", [128, D])
                        ss_f = SB(st, "ss_f", [128, 16])
                        fgbc = SB(st, "fgbc", [128, D])
                        P.dma(lambda e: e.dma_start(out=fgbc[:], in_=fng.partition_broadcast(128)), writes=["fgbc"])
                        pG = [PS(st, "pG%d" % i, [128, 512]) for i in range(2)]
                        pU = [PS(st, "pU%d" % i, [128, 512]) for i in range(2)]
                        pD = [PS(st, "pD%d" % i, [128, D]) for i in range(2)]
                        NE = 65
                        pend = None
                        dcount = [0]

                        def emit_down(e_, g, wi, ai, first):
                            for tt in range(4):
                                di = dcount[0] % 2
                                dcount[0] += 1
                                tile_i = g * 4 + tt
                                for hh in range(2):
                                    hs = slice(hh * 512, (hh + 1) * 512)
                                    for k2 in range(2):
                                        TE(lambda e, di=di, hs=hs, k2=k2, tt=tt: e.matmul(pD[di][:, hs], lhsT=actT[ai][:, k2, tt * 128:(tt + 1) * 128], rhs=wdb[wi][:, k2, hs],
                                                                                         start=(k2 == 0), stop=(k2 == 1)), ["actT%d" % ai, "wdb%d" % wi], [("pD%d" % di, hh)])
                                    gcol = gate[:, tile_i, e_:e_ + 1]
                                    if first:
                                        V(lambda e, di=di, hs=hs, gcol=gcol, tile_i=tile_i: e.tensor_scalar(out=acc[:, tile_i, hs], in0=pD[di][:, hs], scalar1=gcol, scalar2=None, op0=ALU.mult),
                                          [("pD%d" % di, hh), "gate"], [("acc", tile_i * 2 + hh)])
                                    else:
                                        V(lambda e, di=di, hs=hs, gcol=gcol, tile_i=tile_i: e.scalar_tensor_tensor(out=acc[:, tile_i, hs], in0=pD[di][:, hs], scalar=gcol, in1=acc[:, tile_i, hs], op0=ALU.mult, op1=ALU.add),
                                          [("pD%d" % di, hh), "gate", ("acc", tile_i * 2 + hh)], [("acc", tile_i * 2 + hh)])

                        cnt = 0
                        for e_ in range(NE):
                            wi = e_ % 3
                            if e_ < 64:
                                srcs = (ewg[e_], ewu[e_], ewd[e_])
                            else:
                                srcs = (swg, swu, swd)
                            P.dma(lambda e, wi=wi, s=srcs[0]: e.dma_start(out=wgb[wi][:], in_=s.rearrange("(p k) n -> p k n", k=8)), writes=["wgb%d" % wi], eng="gpsimd")
                            P.dma(lambda e, wi=wi, s=srcs[1]: e.dma_start(out=wub[wi][:], in_=s.rearrange("(p k) n -> p k n", k=8)), writes=["wub%d" % wi], eng="gpsimd")
                            P.dma(lambda e, wi=wi, s=srcs[2]: e.dma_start(out=wdb[wi][:], in_=s.rearrange("(k p) n -> p k n", p=128)), writes=["wdb%d" % wi], eng="gpsimd")
                            for g in range(4):
                                ai = cnt % 2
                                cnt += 1
                                gsl = slice(g * 512, (g + 1) * 512)
                                for fc in range(2):
                                    fs = slice(fc * 128, (fc + 1) * 128)
                                    for k in range(8):
                                        TE(lambda e, fc=fc, fs=fs, k=k, gsl=gsl: e.matmul(pG[fc][:], lhsT=wgb[wi][:, k, fs], rhs=h2T[:, k, gsl], start=(k == 0), stop=(k == 7)), ["wgb%d" % wi, "h2T"], ["pG%d" % fc])
                                    for k in range(8):
                                        TE(lambda e, fc=fc, fs=fs, k=k, gsl=gsl: e.matmul(pU[fc][:], lhsT=wub[wi][:, k, fs], rhs=h2T[:, k, gsl], start=(k == 0), stop=(k == 7)), ["wub%d" % wi, "h2T"], ["pU%d" % fc])
                                    S(lambda e, fc=fc: e.activation(out=sgt[fc][:], in_=pG[fc][:], func=AF.Silu), ["pG%d" % fc], ["sgt%d" % fc])
                                    V(lambda e, fc=fc, ai=ai: e.tensor_tensor(out=actT[ai][:, fc, :], in0=pU[fc][:], in1=sgt[fc][:], op=ALU.mult), ["pU%d" % fc, "sgt%d" % fc], [("actT%d" % ai, fc)])
                                if pend is not None:
                                    emit_down(*pend)
                                pend = (e_, g, wi, ai, e_ == 0)
                        emit_down(*pend)
                        if b == 0:
                            dump("acc", acc[:, 0, :], "acc")
                        check("moe")
                        G(lambda e: e.memset(ss_f[:], 0.0), [], ["ss_f"])
                        for ti in range(16):
                            i = ti % 4
                            tsl = slice(ti * 128, (ti + 1) * 128)
                            P.dma(lambda e, i=i, tsl=tsl: e.dma_start(out=xt4[i][:], in_=x1_d[b, tsl, :]), reads=[("x1_d", b * 16 + ti)], writes=["xt4_%d" % i])
                            ak = [("acc", ti * 2), ("acc", ti * 2 + 1)]
                            V(lambda e, ti=ti: e.tensor_tensor(out=acc[:, ti, :], in0=acc[:, ti, :], in1=g2bc[:], op=ALU.mult), ak + ["g2bc"], ak)
                            V(lambda e, i=i, ti=ti: e.tensor_tensor(out=acc[:, ti, :], in0=acc[:, ti, :], in1=xt4[i][:], op=ALU.add), ak + ["xt4_%d" % i], ak)
                            S(lambda e, ti=ti: e.activation(out=junk[:], in_=acc[:, ti, :], func=AF.Square, accum_out=ss_f[:, ti:ti + 1]), ak, ["junk", ("ss_f", ti)])
                        V(lambda e: e.tensor_scalar(out=ss_f[:], in0=ss_f[:], scalar1=1.0 / D, scalar2=1e-6, op0=ALU.mult, op1=ALU.add), ["ss_f"], ["ss_f"])
                        S(lambda e: e.activation(out=ss_f[:], in_=ss_f[:], func=AF.Sqrt), ["ss_f"], ["ss_f"])
                        V(lambda e: e.reciprocal(out=ss_f[:], in_=ss_f[:]), ["ss_f"], ["ss_f"])
                        for ti in range(16):
                            tsl = slice(ti * 128, (ti + 1) * 128)
                            ak = [("acc", ti * 2), ("acc", ti * 2 + 1)]
                            V(lambda e, ti=ti: e.scalar_tensor_tensor(out=acc[:, ti, :], in0=acc[:, ti, :], scalar=ss_f[:, ti:ti + 1], in1=fgbc[:], op0=ALU.mult, op1=ALU.mult), ak + ["ss_f", "fgbc"], ak)
                            P.dma(lambda e, ti=ti, tsl=tsl: e.dma_start(out=out_d[b, tsl, :], in_=acc[:, ti, :]), reads=ak, is_output=True)
                        check("b0done")
                    P.barrier()
        stopped = False
        try:
            _body()
        except _Stop:
            stopped = True
        print("emitted", getattr(P, "nemit", 0), {e: len(v) for e, v in P.items.items()})
        if not getattr(P, "replayed", False):
            P.finish()
            with contextlib.ExitStack() as rs:
                P.replay(rs)
        if stopped:
            top.pop_all()
    return nc


def _host_inputs(inp):
    f = lambda a: np.ascontiguousarray(np.asarray(a, dtype=np.float32))
    x = f(inp["x"]); c = f(inp["c"]); ctx = f(inp["ctx"]); c_ctx = f(inp["c_ctx"])
    pvv = np.zeros((128, NPV), np.float32)
    col = lambda v: f(v).reshape(-1, 128).T
    pvv[:, PV["w0"]:PV["w0"] + 8] = col(inp["rw_w0"][0].reshape(-1))
    pvv[:, PV["a0"]:PV["a0"] + 8] = col(inp["rw_a0"][0].reshape(-1))
    pvv[:, PV["k_k"]:PV["k_k"] + 4] = col(inp["rw_k_k"][0])
    pvv[:, PV["k_a"]:PV["k_a"] + 4] = col(inp["rw_k_a"][0])
    pvv[:, PV["r_k"]:PV["r_k"] + 4] = col(inp["rw_r_k"][0].reshape(-1))
    pvv[:, PV["lnx_g"]:PV["lnx_g"] + 4] = col(inp["rw_lnx_g"][0])
    pvv[:, PV["lnx_b"]:PV["lnx_b"] + 4] = col(inp["rw_lnx_b"][0])
    lbl = f(inp["hg_lb_logits"])
    for d in range(2):
        for s in range(2):
            pvv[:, PV["lbl"] + d * 8 + s * 4:PV["lbl"] + d * 8 + s * 4 + 4] = col(lbl[d, s])
    pvv[:, PV["hgn"]] = np.tile(f(inp["hg_norm_g"][0]), 2)
    pvv[:, PV["n1g"]:PV["n1g"] + 8] = f(inp["norm1_g"][0]).reshape(128, 8)
    pvv[:, PV["n2g"]:PV["n2g"] + 8] = f(inp["norm2_g"][0]).reshape(128, 8)
    convw = f(inp["rw_conv"][0]).reshape(9, 12, 128).transpose(2, 1, 0)
    ident = np.eye(128, dtype=np.float32)
    bd = np.zeros((128, 128), np.float32); bd[:64, :64] = 1; bd[64:, 64:] = 1
    r = np.arange(128)[:, None]; cc = np.arange(128)[None, :]
    su = ((r < cc) * bd).astype(np.float32); sl_ = ((r > cc) * bd).astype(np.float32)
    iu = ((r <= cc) * bd).astype(np.float32); il = ((r >= cc) * bd).astype(np.float32)
    z = np.zeros((128, 128), np.float32)
    masks = np.stack([np.concatenate([su, iu, sl_, z], 1), np.concatenate([sl_, il, su, z], 1)], 0)
    shared = dict(
        w_mod=f(inp["w_mod"][0]), b_mod=f(inp["b_mod"][0]).reshape(1, -1), w_in=f(inp["w_in"][0]), convw=np.ascontiguousarray(convw), pv=pvv,
        w2=f(inp["rw_w2"][0]).reshape(128, 512), a2=f(inp["rw_a2"][0]).reshape(128, 512), g2=f(inp["rw_g2"][0]),
        w_out=f(inp["w_out"][0]), router_w=f(inp["router_w"][0]), router_b=f(inp["router_b"][0]).reshape(1, 64),
        ewg=f(inp["exp_w_gate"][0]), ewu=f(inp["exp_w_up"][0]), ewd=f(inp["exp_w_down"][0]),
        swg=f(inp["sh_w_gate"][0]), swu=f(inp["sh_w_up"][0]), swd=f(inp["sh_w_down"][0]),
        fng=f(inp["final_norm_g"]).reshape(1, -1), ident=ident, bdones=bd, masks=np.ascontiguousarray(masks))
    maps = []
    for core in range(8):
        b0 = core * NB
        cv = np.stack([c[b0], c[b0 + 1], c_ctx], -1).reshape(128, 8, 3)
        m = dict(shared)
        m["x_in"] = np.ascontiguousarray(x[b0:b0 + NB]); m["ctx_in"] = np.ascontiguousarray(ctx[b0:b0 + NB]); m["cvec"] = np.ascontiguousarray(cv)
        maps.append(m)
    return maps


def kernel(**inputs):
    maps = _host_inputs(inputs)
    nc = build_program()
    res = run_bass_kernel_spmd(nc, maps, core_ids=list(range(8)))
    return np.concatenate([r["out"] for r in res.results], axis=0).astype(np.float32)
```
